# Optimizing a Trainium2 kernel written in Bass

```python
import jax, jax.numpy as jnp
from jax import lax
import numpy as np

D_MODEL = 1024
BATCH = 4
SEQ = 4096
DEPTH = 1

N_META = 16
BLOCK = 128
PAD_FRONT = BLOCK - N_META
HEAD_DIM = 64
ATTN_WIDTH = D_MODEL // 2
LRU_WIDTH = D_MODEL - ATTN_WIDTH
N_Q_HEADS = ATTN_WIDTH // HEAD_DIM
N_KV_HEADS = 2
Q_PER_KV = N_Q_HEADS // N_KV_HEADS
KV_WIDTH = N_KV_HEADS * HEAD_DIM
WINDOW = 128
LRU_BLOCKS = 8
LRU_BLOCK_W = LRU_WIDTH // LRU_BLOCKS
CONV_W = 4
LRU_C = 8.0
IN_SPLITS = (ATTN_WIDTH, KV_WIDTH, KV_WIDTH, LRU_WIDTH, LRU_WIDTH)
IN_COLS = ATTN_WIDTH + 2 * KV_WIDTH + 2 * LRU_WIDTH
N_GROUPS = 4
EXPERTS_PER_GROUP = 8
N_EXPERTS = N_GROUPS * EXPERTS_PER_GROUP
TOP_K = 2
D_FF_EXPERT = D_MODEL // 2
MOE_BLOCK = 128
ALPHA = (2.0 * DEPTH) ** 0.25
BETA = (8.0 * DEPTH) ** -0.25
EPS = 1e-5
NEG = -1e30

kernel_name = 'hymba_swa_rglru_hmoe_deepnorm'


def layer_norm(x, g, b):
    xf = x.astype(jnp.float32)
    mu = jnp.mean(xf, axis=-1, keepdims=True)
    var = jnp.mean(jnp.square(xf - mu), axis=-1, keepdims=True)
    return ((xf - mu) * lax.rsqrt(var + EPS) * g.astype(jnp.float32) + b.astype(jnp.float32)).astype(x.dtype)


def rms_norm(x, g):
    xf = x.astype(jnp.float32)
    ms = jnp.mean(jnp.square(xf), axis=-1, keepdims=True)
    return (xf * lax.rsqrt(ms + EPS) * g.astype(jnp.float32)).astype(x.dtype)


def alibi_slopes():
    return jnp.exp2(-8.0 * jnp.arange(1, N_Q_HEADS + 1, dtype=jnp.float32) / N_Q_HEADS)


def sliding_window_attention(q, k, v, sinks):
    bsz, l_len = q.shape[0], q.shape[1]
    nb = l_len // BLOCK
    f32 = jnp.float32
    qb = q.astype(f32).reshape(bsz, nb, BLOCK, N_KV_HEADS, Q_PER_KV, HEAD_DIM)
    kb = k.astype(f32).reshape(bsz, nb, BLOCK, N_KV_HEADS, HEAD_DIM)
    vb = v.astype(f32).reshape(bsz, nb, BLOCK, N_KV_HEADS, HEAD_DIM)
    shift = ((0, 0), (1, 0), (0, 0), (0, 0), (0, 0))
    kw = jnp.concatenate([jnp.pad(kb, shift)[:, :-1], kb], axis=2)
    vw = jnp.concatenate([jnp.pad(vb, shift)[:, :-1], vb], axis=2)
    scores = jnp.einsum('bnqhgd,bnkhd->bnhgqk', qb, kw) * (HEAD_DIM ** -0.5)
    qi = np.arange(BLOCK)[:, None]
    kj = np.arange(2 * BLOCK)[None, :]
    dist = qi - kj + BLOCK
    band = (dist >= 0) & (dist < WINDOW)
    key_pos = np.arange(nb)[:, None] * BLOCK - BLOCK + np.arange(2 * BLOCK)[None, :]
    key_valid = key_pos >= PAD_FRONT
    mask = band[None, :, :] & key_valid[:, None, :]
    slopes = alibi_slopes().reshape(N_KV_HEADS, Q_PER_KV, 1, 1)
    logits = scores - slopes * jnp.asarray(dist, f32)
    logits = jnp.where(mask[None, :, None, None], logits, NEG)
    sink_col = jnp.broadcast_to(sinks.astype(f32).reshape(N_KV_HEADS, Q_PER_KV, 1, 1), logits.shape[:-1] + (1,))
    probs = jax.nn.softmax(jnp.concatenate([logits, sink_col], axis=-1), axis=-1)[..., :-1]
    out = jnp.einsum('bnhgqk,bnkhd->bnqhgd', probs, vw)
    return out.reshape(bsz, l_len, ATTN_WIDTH).astype(q.dtype)


def causal_depthwise_conv(x, w, b):
    y = lax.conv_general_dilated(x, w[:, None, :].astype(x.dtype), window_strides=(1,),
                                 padding=[(CONV_W - 1, 0)],
                                 dimension_numbers=('NWC', 'WIO', 'NWC'),
                                 feature_group_count=x.shape[-1])
    return y + b.astype(x.dtype)


def _linear_combine(c1, c2):
    a1, b1 = c1
    a2, b2 = c2
    return a1 * a2, a2 * b1 + b2


def rg_lru(xc, w_a, b_a, w_x, b_x, lam):
    bsz, t_len, width = xc.shape
    f32 = jnp.float32
    xf = xc.astype(f32)
    xb = xf.reshape(bsz, t_len, LRU_BLOCKS, LRU_BLOCK_W)
    r = jax.nn.sigmoid(jnp.einsum('btnc,ncd->btnd', xb, w_a.astype(f32)) + b_a.astype(f32)).reshape(bsz, t_len, width)
    i = jax.nn.sigmoid(jnp.einsum('btnc,ncd->btnd', xb, w_x.astype(f32)) + b_x.astype(f32)).reshape(bsz, t_len, width)
    log_a = -LRU_C * r * jax.nn.softplus(-lam.astype(f32))
    a = jnp.exp(log_a)
    u = jnp.sqrt(-jnp.expm1(2.0 * log_a)) * (i * xf)
    _, h = lax.associative_scan(_linear_combine, (a, u), axis=1)
    return h.astype(xc.dtype)


def token_mixer(x, w_in, conv_w, conv_b, lru_wa, lru_ba, lru_wx, lru_bx, lru_lambda,
                attn_sinks, g_attn, g_lru, w_out):
    bsz, t_len, _ = x.shape
    proj = jnp.einsum('btd,dc->btc', x, w_in)
    offs = np.cumsum(IN_SPLITS)[:-1].tolist()
    q, k, v, xr, yr = jnp.split(proj, offs, axis=-1)
    pad = ((0, 0), (PAD_FRONT, 0), (0, 0))
    l_len = t_len + PAD_FRONT
    qp = jnp.pad(q, pad).reshape(bsz, l_len, N_Q_HEADS, HEAD_DIM)
    kp = jnp.pad(k, pad).reshape(bsz, l_len, N_KV_HEADS, HEAD_DIM)
    vp = jnp.pad(v, pad).reshape(bsz, l_len, N_KV_HEADS, HEAD_DIM)
    attn = sliding_window_attention(qp, kp, vp, attn_sinks)[:, PAD_FRONT:]
    xc = causal_depthwise_conv(xr, conv_w, conv_b)
    lru = rg_lru(xc, lru_wa, lru_ba, lru_wx, lru_bx, lru_lambda) * jax.nn.gelu(yr)
    merged = jnp.concatenate([rms_norm(attn, g_attn), rms_norm(lru, g_lru)], axis=-1)
    return jnp.einsum('btc,cd->btd', merged, w_out)


def hierarchical_moe(h, w_group, b_group, w_router, b_router, w_gate, w_up, w_down):
    bsz, t_len, d = h.shape
    f32 = jnp.float32
    xf = h.reshape(-1, d)
    n_tok = xf.shape[0]
    x32 = xf.astype(f32)
    g_logits = x32 @ w_group.astype(f32) + b_group.astype(f32)
    g_prob = jax.nn.softmax(g_logits, axis=-1)
    g_idx = jnp.argmax(g_logits, axis=-1)
    g_w = jnp.take_along_axis(g_prob, g_idx[:, None], axis=-1)
    e_logits = jnp.einsum('nd,gde->nge', x32, w_router.astype(f32)) + b_router.astype(f32)
    e_logits = jnp.take_along_axis(e_logits, g_idx[:, None, None], axis=1)[:, 0]
    top_v, top_i = lax.top_k(e_logits, TOP_K)
    gates = g_w * jax.nn.softmax(top_v, axis=-1)
    expert = g_idx[:, None].astype(jnp.int32) * EXPERTS_PER_GROUP + top_i.astype(jnp.int32)
    n_slots = n_tok * TOP_K
    flat_e = expert.reshape(-1)
    flat_tok = jnp.arange(n_slots, dtype=jnp.int32) // TOP_K
    flat_g = gates.reshape(-1)
    order = jnp.argsort(flat_e)
    se = flat_e[order]
    counts = jnp.bincount(flat_e, length=N_EXPERTS).astype(jnp.int32)
    starts = jnp.cumsum(counts) - counts
    padded = (counts + MOE_BLOCK - 1) // MOE_BLOCK * MOE_BLOCK
    pends = jnp.cumsum(padded)
    pstarts = pends - padded
    dest = pstarts[se] + (jnp.arange(n_slots, dtype=jnp.int32) - starts[se])
    n_blocks = -(-n_slots // MOE_BLOCK) + N_EXPERTS
    cap = n_blocks * MOE_BLOCK
    tok_buf = jnp.zeros((cap,), jnp.int32).at[dest].set(flat_tok[order])
    gate_buf = jnp.zeros((cap,), f32).at[dest].set(flat_g[order])
    block_start = jnp.arange(n_blocks, dtype=jnp.int32) * MOE_BLOCK
    block_e = jnp.minimum(jnp.searchsorted(pends, block_start, side='right'), N_EXPERTS - 1)

    def run_block(args):
        toks, e = args
        xb = xf[toks]
        return (jax.nn.silu(xb @ w_gate[e]) * (xb @ w_up[e])) @ w_down[e]

    yb = lax.map(run_block, (tok_buf.reshape(n_blocks, MOE_BLOCK), block_e))
    y = jnp.zeros((n_tok, d), f32).at[tok_buf].add(yb.reshape(cap, d).astype(f32) * gate_buf[:, None])
    return y.reshape(bsz, t_len, d).astype(h.dtype)


def setup_inputs(seed: int = 0) -> dict:
    key = jax.random.key(seed)
    ks = jax.random.split(key, 26)
    f32 = jnp.float32

    def nrm(k, shape, scale):
        return jax.random.normal(k, shape, f32) * scale

    x = nrm(ks[0], (BATCH, SEQ, D_MODEL), 1.0)
    meta_tokens = nrm(ks[1], (N_META, D_MODEL), 1.0)
    col_scale = jnp.concatenate([jnp.ones((ATTN_WIDTH + KV_WIDTH,), f32),
                                 jnp.full((KV_WIDTH,), BETA, f32),
                                 jnp.ones((2 * LRU_WIDTH,), f32)])
    w_in = nrm(ks[2], (DEPTH, D_MODEL, IN_COLS), D_MODEL ** -0.5) * col_scale
    conv_w = nrm(ks[3], (DEPTH, CONV_W, LRU_WIDTH), CONV_W ** -0.5)
    conv_b = nrm(ks[4], (DEPTH, LRU_WIDTH), 0.01)
    lru_wa = nrm(ks[5], (DEPTH, LRU_BLOCKS, LRU_BLOCK_W, LRU_BLOCK_W), LRU_BLOCK_W ** -0.5)
    lru_ba = nrm(ks[6], (DEPTH, LRU_BLOCKS, LRU_BLOCK_W), 0.01)
    lru_wx = nrm(ks[7], (DEPTH, LRU_BLOCKS, LRU_BLOCK_W, LRU_BLOCK_W), LRU_BLOCK_W ** -0.5)
    lru_bx = nrm(ks[8], (DEPTH, LRU_BLOCKS, LRU_BLOCK_W), 0.01)
    base = jax.random.uniform(ks[9], (DEPTH, LRU_WIDTH), f32, 0.9, 0.999)
    lru_lambda = jnp.log(base) - jnp.log1p(-base)
    attn_sinks = nrm(ks[10], (DEPTH, N_Q_HEADS), 1.0)
    g_attn = 1.0 + nrm(ks[11], (DEPTH, ATTN_WIDTH), 0.02)
    g_lru = 1.0 + nrm(ks[12], (DEPTH, LRU_WIDTH), 0.02)
    w_out = nrm(ks[13], (DEPTH, ATTN_WIDTH + LRU_WIDTH, D_MODEL), D_MODEL ** -0.5 * BETA)
    ln1_g = 1.0 + nrm(ks[14], (DEPTH, D_MODEL), 0.02)
    ln1_b = nrm(ks[15], (DEPTH, D_MODEL), 0.01)
    w_group = nrm(ks[16], (DEPTH, D_MODEL, N_GROUPS), D_MODEL ** -0.5)
    b_group = nrm(ks[17], (DEPTH, N_GROUPS), 0.01)
    w_router = nrm(ks[18], (DEPTH, N_GROUPS, D_MODEL, EXPERTS_PER_GROUP), D_MODEL ** -0.5)
    b_router = nrm(ks[19], (DEPTH, N_GROUPS, EXPERTS_PER_GROUP), 0.01)
    w_gate = nrm(ks[20], (DEPTH, N_EXPERTS, D_MODEL, D_FF_EXPERT), D_MODEL ** -0.5)
    w_up = nrm(ks[21], (DEPTH, N_EXPERTS, D_MODEL, D_FF_EXPERT), D_MODEL ** -0.5)
    w_down = nrm(ks[22], (DEPTH, N_EXPERTS, D_FF_EXPERT, D_MODEL), D_FF_EXPERT ** -0.5 * BETA)
    ln2_g = 1.0 + nrm(ks[23], (DEPTH, D_MODEL), 0.02)
    ln2_b = nrm(ks[24], (DEPTH, D_MODEL), 0.01)
    return {'x': x, 'meta_tokens': meta_tokens, 'w_in': w_in, 'conv_w': conv_w, 'conv_b': conv_b,
            'lru_wa': lru_wa, 'lru_ba': lru_ba, 'lru_wx': lru_wx, 'lru_bx': lru_bx,
            'lru_lambda': lru_lambda, 'attn_sinks': attn_sinks, 'g_attn': g_attn, 'g_lru': g_lru,
            'w_out': w_out, 'ln1_g': ln1_g, 'ln1_b': ln1_b, 'w_group': w_group, 'b_group': b_group,
            'w_router': w_router, 'b_router': b_router, 'w_gate': w_gate, 'w_up': w_up,
            'w_down': w_down, 'ln2_g': ln2_g, 'ln2_b': ln2_b}


def reference(x, meta_tokens, w_in, conv_w, conv_b, lru_wa, lru_ba, lru_wx, lru_bx, lru_lambda,
              attn_sinks, g_attn, g_lru, w_out, ln1_g, ln1_b, w_group, b_group, w_router,
              b_router, w_gate, w_up, w_down, ln2_g, ln2_b):
    bsz = x.shape[0]
    meta = jnp.broadcast_to(meta_tokens[None].astype(x.dtype), (bsz, N_META, D_MODEL))
    h = jnp.concatenate([meta, x], axis=1)
    for l in range(DEPTH):
        mix = token_mixer(h, w_in[l], conv_w[l], conv_b[l], lru_wa[l], lru_ba[l], lru_wx[l],
                          lru_bx[l], lru_lambda[l], attn_sinks[l], g_attn[l], g_lru[l], w_out[l])
        h = layer_norm(ALPHA * h + mix, ln1_g[l], ln1_b[l])
        ff = hierarchical_moe(h, w_group[l], b_group[l], w_router[l], b_router[l],
                              w_gate[l], w_up[l], w_down[l])
        h = layer_norm(ALPHA * h + ff, ln2_g[l], ln2_b[l])
    return h[:, N_META:]
```

```python
import contextlib
import numpy as np
import concourse.bass as bass
import concourse.mybir as mybir
from concourse.bass_utils import run_bass_kernel_spmd

F32 = mybir.dt.float32
BF16 = mybir.dt.bfloat16
I32 = mybir.dt.int32
AF = mybir.ActivationFunctionType
ALU = mybir.AluOpType
AX = mybir.AxisListType

D = 1024
NT_MAIN = 16
NT_PRE = 17
NTILES = NT_PRE + NT_MAIN
NB = 48
SBK = 256
ALPHA = 2.0 ** 0.25
EPS = 1e-5
QCOL, KCOL, VCOL, XRCOL, YRCOL = 0, 512, 640, 768, 1280
GELU_C = 1.5957691216057308


class KB:
    def __init__(self, nc, stack):
        self.nc = nc
        self.stack = stack
        self.E = dict(pe=nc.tensor, dve=nc.vector, act=nc.scalar, pool=nc.gpsimd, sp=nc.sync)
        self.sems = {}
        self.cnt = {}
        for e in ("pe", "dve", "act", "pool"):
            self.sems[e] = stack.enter_context(nc.semaphore("sem_" + e))
            self.cnt[e] = 0
        self.seen = {e: {} for e in self.E}
        self.lastw = {}
        self.reads = {}

    def dma_sem(self, name):
        if name not in self.sems:
            self.sems[name] = self.stack.enter_context(self.nc.semaphore("sem_" + name))
            self.cnt[name] = 0
        return name

    def _need(self, reads, writes):
        need = {}

        def add(k, v):
            if v > need.get(k, 0):
                need[k] = v

        for r in reads:
            lw = self.lastw.get(r)
            if lw:
                add(*lw)
        for w in writes:
            lw = self.lastw.get(w)
            if lw:
                add(*lw)
            for k, v in self.reads.get(w, {}).items():
                add(k, v)
        return need

    def _emit_waits(self, eng, need):
        for k, v in need.items():
            if k == eng and eng == "pe":
                continue
            if self.seen[eng].get(k, 0) >= v:
                continue
            self.E[eng].wait_ge(self.sems[k], v)
            self.seen[eng][k] = v

    def sync_reads(self, eng, reads):
        self._emit_waits(eng, self._need(reads, []))

    def _record(self, key, val, reads, writes):
        for r in reads:
            d = self.reads.setdefault(r, {})
            if val > d.get(key, 0):
                d[key] = val
        for w in writes:
            self.lastw[w] = (key, val)
            self.reads[w] = {}

    def op(self, eng, fn, reads=(), writes=()):
        self._emit_waits(eng, self._need(reads, writes))
        inst = fn(self.E[eng])
        self.cnt[eng] += 1
        inst.then_inc(self.sems[eng], 1)
        self._record(eng, self.cnt[eng], reads, writes)
        return inst

    def dma(self, eng, sem, fn, reads=(), writes=()):
        self.dma_sem(sem)
        self._emit_waits(eng, self._need(reads, writes))
        inst = fn(self.E[eng])
        self.cnt[sem] += 16
        inst.then_inc(self.sems[sem], 16)
        self._record(sem, self.cnt[sem], reads, writes)
        return inst

    def seal(self, sems):
        for r, (key, _) in list(self.lastw.items()):
            if key in sems:
                self.lastw[r] = (key, self.cnt[key])

    def barrier(self):
        for eng in self.E:
            for k2, v in self.cnt.items():
                if v and self.seen[eng].get(k2, 0) < v:
                    self.E[eng].wait_ge(self.sems[k2], v)
                    self.seen[eng][k2] = v
        self.lastw = {}
        self.reads = {}


def build_program(stage=3):
    nc = bass.Bass("TRN2", target_bir_lowering=False)

    def din(name, shape, dt=F32):
        return nc.dram_tensor(name, list(shape), dt, kind="ExternalInput").ap()

    xsT = din("xsT", [D, NTILES * 128])
    xm = din("xm", [NT_MAIN * 128, D])
    tmask_d = din("tmask", [128, NT_PRE * 128])
    masks_d = din("masks", [128, 3 * 8 * 128])
    w_in_d = din("w_in", [D, 1792])
    w_out_d = din("w_out", [D, D])
    wbd_d = din("wbd", [128, 2 * 4 * 128])
    pp_d = din("pp", [128, 40])
    rep_d = din("rep", [128, 124])
    ln_d = din("ln", [128, 4 * D])
    w_r_d = din("w_r", [D, 36])
    cf_d = din("cf", [128, 4 * 128])
    w_gate_d = din("w_gate", [32 * 256, 2048])
    w_up_d = din("w_up", [32 * 256, 2048])
    w_down_d = din("w_down", [32 * 256, 2048])
    pcol_d = din("pcol", [128, 2])
    out_d = nc.dram_tensor("out", [NT_MAIN * 128, D], F32, kind="ExternalOutput").ap()
    Xs = nc.dram_tensor("Xs", [NB * SBK, D], BF16, kind="Internal").ap()
    Ys = nc.dram_tensor("Ys", [NB * SBK, D], F32, kind="Internal").ap()
    h1s = nc.dram_tensor("h1s", [NT_MAIN * 128, D], F32, kind="Internal").ap()
    h1bs = nc.dram_tensor("h1bs", [NT_MAIN * 128, D], BF16, kind="Internal").ap()

    with contextlib.ExitStack() as st0:
        k = KB(nc, st0)

        def sbt(stack, name, shape, dt):
            return stack.enter_context(nc.sbuf_tensor("s_" + name, list(shape), dt))

        pb = [st0.enter_context(nc.psum_tensor(f"pb{i}", [128, 512], F32)) for i in range(8)]

        def V(fn, r=(), w=()):
            return k.op("dve", fn, r, w)

        def A(fn, r=(), w=()):
            return k.op("act", fn, r, w)

        def P(fn, r=(), w=()):
            return k.op("pe", fn, r, w)

        def GP(fn, r=(), w=()):
            return k.op("pool", fn, r, w)

        lg_all = sbt(st0, "lg_all", [128, NT_MAIN, 36], F32)
        rep = sbt(st0, "rep", [128, 124], F32)
        cstb = sbt(st0, "cstb", [128, 3, 128], BF16)
        identf = sbt(st0, "identf", [128, 128], F32)
        identb = cstb[:, 0, :]
        lstrict = cstb[:, 1, :]
        onesb = cstb[:, 2, :]

        k.dma("sp", "c0", lambda e: e.dma_start(out=rep[:], in_=rep_d), writes=["rep"])
        k.dma("sp", "c0", lambda e: e.dma_start(out=identf[:], in_=cf_d[:, 0:128]), writes=["identf"])
        k.dma("pool", "c1", lambda e: e.dma_start(out=cstb[:].rearrange("p a d -> p (a d)"), in_=cf_d[:, 128:512]), writes=["cstb"])

        with contextlib.ExitStack() as sa:
            win = sbt(sa, "win", [128, 8, 1792], BF16)
            wo = sbt(sa, "wo", [128, 8, D], BF16)
            lnp = sbt(sa, "lnp", [128, 2, D], F32)
            h1bt = [sbt(sa, f"h1bt{i}", [128, D], BF16) for i in range(2)]
            wbd = sbt(sa, "wbd", [128, 2, 4, 128], BF16)
            ppt = sbt(sa, "ppt", [128, 40], F32)
            sc = sbt(sa, "sc", [128, 4], F32)
            nba = sbt(sa, "nba", [128, 8], F32)
            esink = sbt(sa, "esink", [128, 8], F32)
            wr = sbt(sa, "wr", [128, 8, 36], F32)
            msk = sbt(sa, "msk", [128, 3, 8, 128], BF16)
            zt = sbt(sa, "zt", [128, D], BF16)
            xT = [sbt(sa, f"xT{i}", [128, 8, 512], BF16) for i in range(2)]
            xres = [sbt(sa, f"xres{i}", [128, D], F32) for i in range(2)]
            tmask = xres[0]
            qT = sbt(sa, "qT", [128, 4, 512], BF16)
            kT = sbt(sa, "kT", [128, NT_MAIN + 1, 128], BF16)
            vbuf = sbt(sa, "vbuf", [128, NT_MAIN + 1, 2, 65], BF16)
            xr4 = sbt(sa, "xr4", [128, 4, 3 + 512], F32)
            xrh = sbt(sa, "xrh", [128, 4, 3], F32)
            hlast = sbt(sa, "hlast", [128, 4], F32)
            xc4 = sbt(sa, "xc4", [128, 4, 512], F32)
            xcb4 = sbt(sa, "xcb4", [128, 4, 512], BF16)
            ra4 = sbt(sa, "ra4", [128, 4, 512], F32)
            ts4 = sbt(sa, "ts4", [128, 4, 512], F32)
            iu4 = sbt(sa, "iu4", [128, 4, 512], F32)
            lruf = sbt(sa, "lruf", [128, 4, 512], F32)
            sqb = sbt(sa, "sqb", [128, 4, 512], BF16)
            slbc = sbt(sa, "slbc", [128, 512], F32)
            lruT = sbt(sa, "lruT", [128, 4, 512], BF16)
            ebuf = [sbt(sa, f"ebuf{i}", [128, 512], F32) for i in range(2)]
            pT = [sbt(sa, f"pT{i}", [128, 2, 2, 4, 128], BF16) for i in range(1)]
            zz = sbt(sa, "zz", [128, 8], F32)
            rz = sbt(sa, "rz", [128, 8], F32)
            attnf = sbt(sa, "attnf", [128, 512], F32)
            junk = sbt(sa, "junk", [128, 512], BF16)
            sml = sbt(sa, "sml", [128, 16], F32)
            attnb = sbt(sa, "attnb", [128, 512], BF16)
            attnT = [sbt(sa, f"attnT{i}", [128, 4, 128], BF16) for i in range(4)]
            zbs = [sbt(sa, f"zb{i}", [128, D], F32) for i in range(2)]
            zb = zbs[0]
            h1fs = [sbt(sa, f"h1f{i}", [128, D], F32) for i in range(2)]
            h1f = h1fs[0]
            h1T = sbt(sa, "h1T", [128, 4, 128], F32)
            stats = sbt(sa, "stats", [128, 2, 6], F32)
            mv = sbt(sa, "mv", [128, 2], F32)

            k.dma("pool", "c1", lambda e: e.dma_start(out=win[:], in_=w_in_d.rearrange("(c p) n -> p c n", p=128)), writes=["win"])
            k.dma("pool", "c1", lambda e: e.dma_start(out=wbd[:].rearrange("p a c d -> p (a c d)"), in_=wbd_d), writes=["wbd"])
            k.dma("sp", "c0", lambda e: e.dma_start(out=ppt[:], in_=pp_d), writes=["ppt"])
            k.dma("sp", "c0", lambda e: e.dma_start(out=wr[:], in_=w_r_d.rearrange("(c p) n -> p c n", p=128)), writes=["wr"])
            k.dma("pool", "c1", lambda e: e.dma_start(out=msk[:].rearrange("p a h q -> p (a h q)"), in_=masks_d, max_dma_last_dim=4096), writes=["msk"])
            k.dma("sp", "c0", lambda e: e.dma_start(out=lnp[:].rearrange("p a d -> p (a d)"), in_=ln_d[:, 0:2 * D]), writes=["lnp"])
            k.seal(("c0", "c1"))
            wstage = [zb, h1f]
            for c in range(8):
                ws, wsn = wstage[c % 2], (["zb0", "zb1"], ["h1f"])[c % 2]
                k.dma("sp", f"wst{c%2}", lambda e: e.dma_start(out=ws[:], in_=w_out_d[c * 128:(c + 1) * 128, :]), writes=wsn)
                V(lambda e: e.tensor_scalar(out=wo[:, c, :], in0=ws[:], scalar1=ppt[:, 32 + c:33 + c], scalar2=None, op0=ALU.mult),
                  r=wsn + ["ppt"], w=["wo"])
            A(lambda e: e.activation(out=sc[:], in_=ppt[:, 28:32], func=AF.Exp, scale=-1.0), r=["ppt"], w=["sc"])
            A(lambda e: e.activation(out=sc[:], in_=sc[:], func=AF.Ln, bias=1.0, scale=1.0), r=["sc"], w=["sc"])
            V(lambda e: e.tensor_scalar(out=sc[:], in0=sc[:], scalar1=-8.0, scalar2=None, op0=ALU.mult), r=["sc"], w=["sc"])
            A(lambda e: e.activation(out=esink[:], in_=rep[:, 0:8], func=AF.Exp), r=["rep"], w=["esink"])
            V(lambda e: e.memset(zt[:], 0.0), w=["zt"])
            V(lambda e: e.memset(vbuf[:], 1.0), w=["vbuf"])
            V(lambda e: e.memset(xrh[:], 0.0), w=["xrh"])
            V(lambda e: e.memset(hlast[:], 0.0), w=["hlast0", "hlast2"])

            xsT_v = xsT.rearrange("(c p) t -> p c t", p=128)
            pcount = [0]

            def next_pb12():
                pcount[0] += 1
                i = 1 + (pcount[0] % 2)
                return pb[i], f"pb{i}"

            def proj_fm(col0, N, xt, xtn):
                ps, psn = next_pb12()
                for c in range(8):
                    P(lambda e: e.matmul(ps[:, 0:N], lhsT=win[:, c, col0:col0 + 128], rhs=xt[:, c, 0:N], start=(c == 0), stop=(c == 7)),
                      r=["win", xtn], w=[psn])
                return ps, psn

            gi = [0]
            sc_cnt = [0]

            pb_rot = [1, 2, 3, 4]

            def next_pb14():
                pcount[0] += 1
                i = pb_rot[pcount[0] % len(pb_rot)]
                return pb[i], f"pb{i}"

            def proj4(col0, N, xt, xtn):
                ps, psn = next_pb14()
                for c in range(8):
                    P(lambda e: e.matmul(ps[:, 0:N], lhsT=win[:, c, col0:col0 + 128], rhs=xt[:, c, 0:N], start=(c == 0), stop=(c == 7)),
                      r=["win", xtn], w=[psn])
                return ps, psn

            XR = [f"xr4_{j}" for j in range(4)]
            XC = [f"xc4_{j}" for j in range(4)]
            RA = [f"ra4_{j}" for j in range(4)]
            IU = [f"iu4_{j}" for j in range(4)]
            TS = [f"ts4_{j}" for j in range(4)]

            GROUPS = [(g0 * 4, 4, "prefix") for g0 in range(4)] + [(16, 1, "halo")] + [(NT_PRE + g0 * 4, 4, "main") for g0 in range(4)]

            def load_xT(gidx):
                t0_, G_, _ = GROUPS[gidx]
                sl = gidx % 2
                k.dma("pool", f"ldx{sl}", lambda e: e.dma_start(out=xT[sl][:, :, 0:128 * G_], in_=xsT_v[:, :, t0_ * 128:t0_ * 128 + 128 * G_]),
                      writes=[f"xT{sl}"])

            def do_group(t0, G, kind):
                N = 128 * G
                slot = gi[0] % 2
                gi[0] += 1
                xt, xtn = xT[slot], f"xT{slot}"
                if gi[0] == 1:
                    load_xT(0)
                if gi[0] < len(GROUPS):
                    load_xT(gi[0])
                main = kind == "main"
                if main:
                    nz = NB * SBK // 128
                    gq = (t0 - NT_PRE) // 4
                    for i in range(gq * nz // 4, (gq + 1) * nz // 4):
                        k.dma("sp", "xz", lambda e: e.dma_start(out=Xs[i * 128:(i + 1) * 128, :], in_=zt[:]),
                              reads=["zt"], writes=["Xs"])
                use_mask = (kind == "halo") or (kind == "prefix" and t0 == 0)
                if use_mask:
                    k.dma("sp", "ldtm", lambda e: e.dma_start(out=tmask[:, 0:N], in_=tmask_d[:, t0 * 128:t0 * 128 + N]), writes=["xres0"])
                pb_rot[:] = [1, 2, 7] if main else [1, 2, 3, 4]

                def p0():
                    V(lambda e: e.tensor_copy(out=xr4[:, :, 0:3], in_=xrh[:]), r=["xrh", "xrh0", "xrh2"], w=["xr4h"])
                    for j in range(4):
                        ps, psn = proj4(XRCOL + j * 128, N, xt, xtn)
                        A(lambda e: e.copy(out=xr4[:, j, 3:3 + N], in_=ps[:, 0:N]), r=[psn], w=[XR[j]])
                    if main:
                        for g in range(4):
                            ps, psn = proj4(QCOL + g * 128, N, xt, xtn)
                            A(lambda e: e.copy(out=qT[:, g, 0:N], in_=ps[:, 0:N]), r=[psn], w=["qT"])
                    if kind in ("main", "halo"):
                        ps, psn = proj4(KCOL, N, xt, xtn)
                        ks0 = t0 - (NT_PRE - 1)
                        A(lambda e: e.copy(out=kT[:, ks0:ks0 + G, :], in_=ps[:, 0:N].rearrange("p (g t) -> p g t", g=G)),
                          r=[psn], w=[f"kT{ks0 + j}" for j in range(G)])
                        for tl in range(G):
                            ps, psn = next_pb14()
                            for c in range(8):
                                P(lambda e: e.matmul(ps[:, 0:128], lhsT=xt[:, c, tl * 128:(tl + 1) * 128], rhs=win[:, c, VCOL:VCOL + 128],
                                                     start=(c == 0), stop=(c == 7)), r=["win", xtn], w=[psn])
                            A(lambda e: e.copy(out=vbuf[:, ks0 + tl, :, 0:64], in_=ps[:, 0:128].rearrange("p (a d) -> p a d", a=2)),
                              r=[psn], w=[f"v{ks0 + tl}"])

                def p1(j0, nj):
                    J = slice(j0, j0 + nj)
                    for j in range(j0, j0 + nj):
                        V(lambda e: e.tensor_scalar(out=xc4[:, j, 0:N], in0=xr4[:, j, 0:N], scalar1=ppt[:, j * 4:j * 4 + 1],
                                                    scalar2=ppt[:, 16 + j:17 + j], op0=ALU.mult, op1=ALU.add), r=[XR[j], "xr4h", "ppt"], w=[XC[j]])
                        for tap in (1, 2, 3):
                            V(lambda e: e.scalar_tensor_tensor(out=xc4[:, j, 0:N], in0=xr4[:, j, tap:tap + N], scalar=ppt[:, j * 4 + tap:j * 4 + tap + 1],
                                                               in1=xc4[:, j, 0:N], op0=ALU.mult, op1=ALU.add), r=[XR[j], "xr4h", XC[j], "ppt"], w=[XC[j]])
                    V(lambda e: e.tensor_copy(out=xrh[:, J, :], in_=xr4[:, J, N:N + 3]), r=XR[J] + ["xr4h"], w=[f"xrh{j0}"])
                    A(lambda e: e.copy(out=xcb4[:, J, 0:N], in_=xc4[:, J, 0:N]), r=XC[J], w=[f"xcb4_{j0}"])

                def p2(j0, nj):
                    for j in range(j0, j0 + nj):
                        psr, psrn = next_pb14()
                        P(lambda e: e.matmul(psr[:, 0:N], lhsT=wbd[:, 0, j, :], rhs=xcb4[:, j, 0:N], start=True, stop=True), r=["wbd", f"xcb4_{j0}"], w=[psrn])
                        A(lambda e: e.activation(out=ra4[:, j, 0:N], in_=psr[:, 0:N], func=AF.Sigmoid, bias=ppt[:, 20 + j:21 + j]), r=[psrn, "ppt"], w=[RA[j]])
                        psi, psin = next_pb14()
                        P(lambda e: e.matmul(psi[:, 0:N], lhsT=wbd[:, 1, j, :], rhs=xcb4[:, j, 0:N], start=True, stop=True), r=["wbd", f"xcb4_{j0}"], w=[psin])
                        A(lambda e: e.activation(out=iu4[:, j, 0:N], in_=psi[:, 0:N], func=AF.Sigmoid, bias=ppt[:, 24 + j:25 + j]), r=[psin, "ppt"], w=[IU[j]])
                    for j in range(j0, j0 + nj):
                        A(lambda e: e.activation(out=ra4[:, j, 0:N], in_=ra4[:, j, 0:N], func=AF.Exp, scale=sc[:, j:j + 1]), r=[RA[j], "sc"], w=[RA[j]])

                def p3(j0, nj):
                    J = slice(j0, j0 + nj)
                    V(lambda e: e.scalar_tensor_tensor(out=ts4[:, J, 0:N], in0=ra4[:, J, 0:N], scalar=0.99999994, in1=ra4[:, J, 0:N], op0=ALU.min, op1=ALU.mult),
                      r=RA[J], w=TS[J])
                    A(lambda e: e.activation(out=ts4[:, J, 0:N], in_=ts4[:, J, 0:N], func=AF.Ln, bias=1.0, scale=-1.0), r=TS[J], w=TS[J])
                    A(lambda e: e.activation(out=ts4[:, J, 0:N], in_=ts4[:, J, 0:N], func=AF.Exp, scale=0.5), r=TS[J], w=TS[J])
                    V(lambda e: e.tensor_tensor(out=iu4[:, J, 0:N], in0=iu4[:, J, 0:N], in1=xc4[:, J, 0:N], op=ALU.mult), r=IU[J] + XC[J], w=IU[J])
                    V(lambda e: e.tensor_tensor(out=iu4[:, J, 0:N], in0=iu4[:, J, 0:N], in1=ts4[:, J, 0:N], op=ALU.mult), r=IU[J] + TS[J], w=IU[J])
                    if use_mask:
                        tmb = tmask[:, 0:N].unsqueeze(1).to_broadcast([128, nj, N])
                        V(lambda e: e.tensor_tensor(out=ra4[:, J, 0:N], in0=ra4[:, J, 0:N], in1=tmb, op=ALU.mult), r=RA[J] + ["xres0"], w=RA[J])
                        V(lambda e: e.tensor_tensor(out=iu4[:, J, 0:N], in0=iu4[:, J, 0:N], in1=tmb, op=ALU.mult), r=IU[J] + ["xres0"], w=IU[J])

                def p4(j0, nj):
                    J = slice(j0, j0 + nj)
                    for j in range(j0, j0 + nj):
                        V(lambda e: e.tensor_tensor_scan(out=ts4[:, j, 0:N], data0=ra4[:, j, 0:N], data1=iu4[:, j, 0:N], initial=hlast[:, j:j + 1],
                                                         op0=ALU.mult, op1=ALU.add), r=[RA[j], IU[j], TS[j], f"hlast{j0}"], w=[TS[j]])
                    V(lambda e: e.tensor_copy(out=hlast[:, J].unsqueeze(2), in_=ts4[:, J, N - 1:N]), r=TS[J], w=[f"hlast{j0}"])
                    if not main:
                        return
                    for j in range(j0, j0 + nj):
                        psy, psyn = proj4(YRCOL + j * 128, N, xt, xtn)
                        A(lambda e: e.copy(out=xr4[:, j, 3:3 + N], in_=psy[:, 0:N]), r=[psyn], w=[XR[j]])
                    yy = xr4[:, J, 3:3 + N]
                    t2 = xc4[:, J, 0:N]
                    V(lambda e: e.tensor_tensor(out=t2, in0=yy, in1=yy, op=ALU.mult), r=XR[J], w=XC[J])
                    V(lambda e: e.tensor_scalar(out=t2, in0=t2, scalar1=0.044715, scalar2=1.0, op0=ALU.mult, op1=ALU.add), r=XC[J], w=XC[J])
                    V(lambda e: e.tensor_tensor(out=t2, in0=t2, in1=yy, op=ALU.mult), r=XC[J] + XR[J], w=XC[J])
                    A(lambda e: e.activation(out=t2, in_=t2, func=AF.Sigmoid, scale=GELU_C), r=XC[J], w=XC[J])
                    V(lambda e: e.tensor_tensor(out=t2, in0=t2, in1=yy, op=ALU.mult), r=XC[J] + XR[J], w=XC[J])
                    V(lambda e: e.tensor_tensor(out=lruf[:, J, 0:N], in0=t2, in1=ts4[:, J, 0:N], op=ALU.mult), r=XC[J] + TS[J], w=[f"lruf{j0}"])
                    A(lambda e: e.activation(out=sqb[:, J, 0:N], in_=lruf[:, J, 0:N], func=AF.Square), r=[f"lruf{j0}"], w=[f"sqb{j0}"])

                def p5():
                    ps, psn = next_pb14()
                    for j in range(4):
                        P(lambda e: e.matmul(ps[:, 0:N], lhsT=onesb, rhs=sqb[:, j, 0:N], start=(j == 0), stop=(j == 3)), r=["cstb", "sqb0", "sqb2"], w=[psn])
                    A(lambda e: e.activation(out=slbc[:, 0:N], in_=ps[:, 0:N], func=AF.Ln, bias=EPS, scale=1.0 / 512), r=[psn], w=["slbc"])
                    A(lambda e: e.activation(out=slbc[:, 0:N], in_=slbc[:, 0:N], func=AF.Exp, scale=-0.5), r=["slbc"], w=["slbc"])
                    V(lambda e: e.tensor_tensor(out=lruT[:, :, 0:N], in0=lruf[:, :, 0:N], in1=slbc[:, 0:N].unsqueeze(1).to_broadcast([128, 4, N]), op=ALU.mult),
                      r=["lruf0", "lruf2", "slbc"], w=["lruT"])


                HALVES = ((0, 2), (2, 2))
                if not main:
                    p0()
                    for piece in (p1, p2, p3, p4):
                        for j0_, nj_ in HALVES:
                            piece(j0_, nj_)
                    return

                def tileA(tl):
                    ti = t0 + tl - NT_PRE
                    kcur = ti + 1
                    pt_, ptn = pT[0], "pT0"
                    for kvh in range(2):
                        for kt in range(2):
                            ksl = kcur - 1 + kt
                            which = (2 if ti == 0 else 1) if kt == 0 else 0
                            sc_cnt[0] += 1
                            si = 3 + sc_cnt[0] % 2
                            ps, psn = pb[si], f"pb{si}"
                            P(lambda e: e.matmul(ps[:, 0:512].rearrange("p (g q) -> p g q", g=4), lhsT=kT[kvh * 64:(kvh + 1) * 64, ksl, :],
                                                 rhs=qT[kvh * 64:(kvh + 1) * 64, :, tl * 128:(tl + 1) * 128], start=True, stop=True),
                              r=[f"kT{ksl}", "qT"], w=[psn])
                            eb_, ebn = ebuf[sc_cnt[0] % 2], f"ebuf{sc_cnt[0] % 2}"
                            A(lambda e: e.activation(out=eb_[:], in_=ps[:, 0:512], func=AF.Exp, scale=0.125), r=[psn], w=[ebn])
                            V(lambda e: e.tensor_tensor(out=pt_[:, kt, kvh, :, :], in0=eb_[:].rearrange("p (g q) -> p g q", g=4),
                                                        in1=msk[:, which, kvh * 4:(kvh + 1) * 4, :], op=ALU.mult), r=[ebn, "msk"], w=[f"{ptn}_{kt}{kvh}"])
                    for kvh in range(2):
                        ps, psn = pb[5 + kvh], f"pb{5 + kvh}"
                        for g in range(4):
                            for kt in range(2):
                                ksl = kcur - 1 + kt
                                P(lambda e: e.matmul(ps[:, g * 65:(g + 1) * 65], lhsT=pt_[:, kt, kvh, g, :], rhs=vbuf[:, ksl, kvh, :],
                                                     start=(kt == 0), stop=(kt == 1)), r=[f"{ptn}_{kt}{kvh}", f"v{ksl}", "vbuf"], w=[psn])
                        psv = ps[:, 0:260].rearrange("p (g d) -> p g d", g=4)
                        V(lambda e: e.tensor_tensor(out=zz[:, kvh * 4:(kvh + 1) * 4].unsqueeze(2), in0=psv[:, :, 64:65],
                                                    in1=esink[:, kvh * 4:(kvh + 1) * 4].unsqueeze(2), op=ALU.add), r=[psn, "esink"], w=[f"zz{kvh}"])
                        V(lambda e: e.reciprocal(out=rz[:, kvh * 4:(kvh + 1) * 4], in_=zz[:, kvh * 4:(kvh + 1) * 4]), r=[f"zz{kvh}"], w=[f"rz{kvh}"])
                        V(lambda e: e.tensor_tensor(out=attnf[:, kvh * 256:(kvh + 1) * 256].rearrange("p (g d) -> p g d", g=4), in0=psv[:, :, 0:64],
                                                    in1=rz[:, kvh * 4:(kvh + 1) * 4].unsqueeze(2).to_broadcast([128, 4, 64]), op=ALU.mult),
                          r=[psn, f"rz{kvh}"], w=[f"attnf{kvh}"])
                    A(lambda e: e.activation(out=junk[:], in_=attnf[:], func=AF.Square, accum_out=sml[:, 0:1]), r=["attnf0", "attnf1"], w=["junk", "sml0"])
                    A(lambda e: e.activation(out=sml[:, 1:2], in_=sml[:, 0:1], func=AF.Ln, bias=EPS, scale=1.0 / 512), r=["sml0"], w=["sml1"])
                    A(lambda e: e.activation(out=sml[:, 2:3], in_=sml[:, 1:2], func=AF.Exp, scale=-0.5), r=["sml1"], w=["sml2"])
                    V(lambda e: e.tensor_scalar(out=attnb[:], in0=attnf[:], scalar1=sml[:, 2:3], scalar2=None, op0=ALU.mult),
                      r=["attnf0", "attnf1", "sml2"], w=["attnb"])
                    pbt = pb[0][:].bitcast(BF16)
                    for j in range(4):
                        P(lambda e: e.transpose(out=pbt[:, j * 128:(j + 1) * 128], in_=attnb[:, j * 128:(j + 1) * 128], identity=identb),
                          r=["attnb", "cstb"], w=["pb0"])
                    A(lambda e: e.copy(out=attnT[tl][:].rearrange("p j t -> p (j t)"), in_=pbt[:, 0:512]), r=["pb0"], w=[f"attnT{tl}"])

                def tileB(tl):
                    ti = t0 + tl - NT_PRE
                    zb = zbs[ti % 2]
                    h1f = h1fs[ti % 2]
                    H1, H1B = f"h1f{ti % 2}", f"h1fb{ti % 2}"
                    Z0, Z1 = f"zb{ti % 2}_0", f"zb{ti % 2}_1"
                    xr_, xrn = xres[ti % 2], f"xres{ti % 2}"
                    at_, atn = attnT[tl], f"attnT{tl}"
                    for hv in range(2):
                        ps, psn = (pb[7], "pb7") if (2 * ti + hv) % 2 == 0 else (pb[1], "pb1")
                        for j in range(4):
                            P(lambda e: e.matmul(ps[:, 0:512], lhsT=at_[:, j, :], rhs=wo[:, j, hv * 512:(hv + 1) * 512], start=(j == 0), stop=False),
                              r=[atn, "wo"], w=[psn])
                        for j in range(4):
                            P(lambda e: e.matmul(ps[:, 0:512], lhsT=lruT[:, j, tl * 128:(tl + 1) * 128], rhs=wo[:, 4 + j, hv * 512:(hv + 1) * 512],
                                                 start=False, stop=(j == 3)), r=["lruT", "wo"], w=[psn])
                        V(lambda e: e.scalar_tensor_tensor(out=zb[:, hv * 512:(hv + 1) * 512], in0=xr_[:, hv * 512:(hv + 1) * 512], scalar=ALPHA,
                                                           in1=ps[:, 0:512], op0=ALU.mult, op1=ALU.add), r=[xrn, psn], w=[(Z0, Z1)[hv]])
                        V(lambda e: e.bn_stats(out=stats[:, hv, :], in_=zb[:, hv * 512:(hv + 1) * 512]), r=[(Z0, Z1)[hv]], w=[f"stats{hv}"])
                    V(lambda e: e.bn_aggr(out=mv[:], in_=stats[:].rearrange("p a s -> p (a s)")), r=["stats0", "stats1"], w=["mv"])
                    A(lambda e: e.activation(out=sml[:, 3:4], in_=mv[:, 1:2], func=AF.Ln, bias=EPS, scale=1.0), r=["mv"], w=["sml3"])
                    A(lambda e: e.activation(out=sml[:, 4:5], in_=sml[:, 3:4], func=AF.Exp, scale=-0.5), r=["sml3"], w=["sml4"])
                    V(lambda e: e.scalar_tensor_tensor(out=sml[:, 5:6], in0=mv[:, 0:1], scalar=-1.0, in1=sml[:, 4:5], op0=ALU.mult, op1=ALU.mult),
                      r=["mv", "sml4"], w=["sml5"])
                    A(lambda e: e.activation(out=zb[:], in_=zb[:], func=AF.Identity, bias=sml[:, 5:6], scale=sml[:, 4:5]),
                      r=[Z0, Z1, "sml4", "sml5"], w=[Z0, Z1])
                    V(lambda e: e.tensor_tensor(out=zb[:], in0=zb[:], in1=lnp[:, 0, :], op=ALU.mult), r=[Z0, Z1, "lnp"], w=[Z0, Z1])
                    GP(lambda e: e.tensor_tensor(out=h1f[:, 512:1024], in0=zb[:, 512:1024], in1=lnp[:, 1, 512:1024], op=ALU.add), r=[Z0, Z1, "lnp"], w=[H1B])
                    V(lambda e: e.tensor_tensor(out=h1f[:, 0:512], in0=zb[:, 0:512], in1=lnp[:, 1, 0:512], op=ALU.add), r=[Z0, Z1, "lnp"], w=[H1])
                    A(lambda e: e.copy(out=h1bt[ti % 2][:], in_=h1f[:]), r=[H1, H1B], w=[f"h1bt{ti % 2}"])
                    k.dma("act", f"h1bst{ti % 2}", lambda e: e.dma_start(out=h1bs[ti * 128:(ti + 1) * 128, :], in_=h1bt[ti % 2][:]),
                          reads=[f"h1bt{ti % 2}"], writes=["h1bs"])
                    k.dma("pool", "h1st", lambda e: e.dma_start(out=h1s[ti * 128:(ti + 1) * 128, :], in_=h1f[:]), reads=[H1, H1B], writes=["h1s"])
                    if stage == 1:
                        k.dma("sp", "outst", lambda e: e.dma_start(out=out_d[ti * 128:(ti + 1) * 128, :], in_=h1f[:]), reads=[H1, H1B], writes=["out"])
                    for half in range(2):
                        for c in range(4):
                            cc = half * 4 + c
                            P(lambda e: e.transpose(out=pb[0][:, c * 128:(c + 1) * 128], in_=h1f[:, cc * 128:(cc + 1) * 128], identity=identf[:]),
                              r=[H1, H1B, "identf"], w=["pb0"])
                        A(lambda e: e.copy(out=h1T[:].rearrange("p c t -> p (c t)"), in_=pb[0][:, 0:512]), r=["pb0"], w=["h1T"])
                        for c in range(4):
                            cc = half * 4 + c
                            P(lambda e: e.matmul(pb[6][:, 260:296], lhsT=h1T[:, c, :], rhs=wr[:, cc, :], start=(cc == 0), stop=(cc == 7)),
                              r=["h1T", "wr"], w=["pb6"])
                    V(lambda e: e.tensor_tensor(out=lg_all[:, ti, :], in0=pb[6][:, 260:296], in1=rep[:, 8:44], op=ALU.add), r=["pb6", "rep"], w=["lg_all"])

                def ld_xres(tl):
                    ti = t0 + tl - NT_PRE
                    k.dma("sp", f"ldr{ti % 2}", lambda e: e.dma_start(out=xres[ti % 2][:], in_=xm[ti * 128:(ti + 1) * 128, :]), writes=[f"xres{ti % 2}"])

                p0()
                for j0_, nj_ in HALVES:
                    p1(j0_, nj_)
                tileA(0)
                for j0_, nj_ in HALVES:
                    p2(j0_, nj_)
                tileA(1)
                for j0_, nj_ in HALVES:
                    p3(j0_, nj_)
                tileA(2)
                for j0_, nj_ in HALVES:
                    p4(j0_, nj_)
                tileA(3)
                p5()
                ld_xres(0)
                ld_xres(1)
                for tl in range(G):
                    tileB(tl)
                    if tl + 2 < G:
                        ld_xres(tl + 2)

            for grp in GROUPS:
                do_group(*grp)
            k.barrier()

        if stage == 1:
            k.barrier()
            return nc

        T = NT_MAIN
        gates = sbt(st0, "gates", [128, 2, T], F32)
        dest_i = sbt(st0, "dest_i", [128, 2, T], I32)
        idxw = sbt(st0, "idxw", [128, 2, NB], I32)
        pcol = sbt(st0, "pcol", [128, 2], F32)
        k.dma("sp", "c0", lambda e: e.dma_start(out=pcol[:], in_=pcol_d), writes=["pcol"])
        NWB = 3
        wg = [sbt(st0, f"wg{i}", [128, 8, 512], BF16) for i in range(NWB)]
        wu = [sbt(st0, f"wu{i}", [128, 8, 512], BF16) for i in range(NWB)]
        wd = [sbt(st0, f"wd{i}", [128, 4, D], BF16) for i in range(NWB)]
        xb = [sbt(st0, f"xb{i}", [128, D], BF16) for i in range(3)]
        xbT = [sbt(st0, f"xbT{i}", [128, 8, 128], BF16) for i in range(3)]
        sgm = sbt(st0, "sgm", [128, 512], F32)
        gsm = sbt(st0, "gsm", [128, 512], F32)
        ab = [sbt(st0, f"ab{i}", [128, 512], BF16) for i in range(2)]
        aT = [sbt(st0, f"aT{i}", [128, 4, 128], BF16) for i in range(2)]
        yb = [sbt(st0, f"yb{i}", [128, D], F32) for i in range(2)]
        y0 = [sbt(st0, f"y0_{i}", [128, D], F32) for i in range(2)]
        y1 = [sbt(st0, f"y1_{i}", [128, D], F32) for i in range(2)]
        hr = [sbt(st0, f"hr{i}", [128, D], F32) for i in range(2)]
        zcb = [sbt(st0, f"zc{i}", [128, D], F32) for i in range(3)]
        zn = [sbt(st0, f"zn{i}", [128, D], F32) for i in range(2)]
        oc = [sbt(st0, f"oc{i}", [128, D], F32) for i in range(2)]
        stats2 = [sbt(st0, f"stats2_{i}", [128, 2, 6], F32) for i in range(2)]
        mv2 = [sbt(st0, f"mv2_{i}", [128, 2], F32) for i in range(2)]
        sm2 = [sbt(st0, f"sm2_{i}", [128, 4], F32) for i in range(2)]
        lnp2 = sbt(st0, "lnp2", [128, 2, D], F32)
        k.dma("sp", "c2", lambda e: e.dma_start(out=lnp2[:].rearrange("p a d -> p (a d)"), in_=ln_d[:, 2 * D:4 * D]), writes=["lnp2"])

        bnd_reg = nc.gpsimd.alloc_register("wbound")
        nc.gpsimd.reg_mov(bnd_reg, 32 * 256 - 1)

        def issue_weights(pos):
            sbk, ws = sb_order[pos], pos % NWB
            for wt, wsrc, wn in ((wg, w_gate_d, "wg"), (wu, w_up_d, "wu"), (wd, w_down_d, "wd")):
                dst = wt[ws][:].rearrange("p c f -> p (c f)")
                for a in range(2):
                    k.dma("pool", f"wq{wn}{ws}_{a}", lambda e: e.indirect_dma_start(out=dst[:, a * 2048:(a + 1) * 2048], out_offset=None, in_=wsrc,
                                                                                 in_offset=bass.IndirectOffsetOnAxis(ap=idxw[:, a, sbk:sbk + 1], axis=0),
                                                                                 bounds_check=bnd_reg, oob_is_err=False),
                          reads=["idxw"], writes=[f"{wn}{ws}_{a}"])

        sb_order = []
        for i_ in range(NB // 2):
            sb_order += [i_, NB - 1 - i_]

        with contextlib.ExitStack() as sb_:
            def t512(name, dt=F32):
                return sbt(sb_, name, [128, 512], dt)

            gmax = sbt(sb_, "gmax", [128, T], F32)
            goh = sbt(sb_, "goh", [128, T, 4], F32)
            gd = sbt(sb_, "gd", [128, T, 4], F32)
            gs = sbt(sb_, "gs", [128, T], F32)
            gw = sbt(sb_, "gw", [128, T], F32)
            tmp = t512("tmp")
            sel = sbt(sb_, "sel", [128, T, 8], F32)
            sel2 = sbt(sb_, "sel2", [128, T, 8], F32)
            m1 = sbt(sb_, "m1", [128, T], F32)
            m2 = sbt(sb_, "m2", [128, T], F32)
            oh1 = sbt(sb_, "oh1", [128, T, 8], F32)
            oh2 = sbt(sb_, "oh2", [128, T, 8], F32)
            dm = sbt(sb_, "dm", [128, T], F32)
            OH1 = t512("OH1")
            OH2 = t512("OH2")
            OHb = t512("OHb", BF16)
            tot_et = t512("tot_et")
            flags = t512("flags")
            cum_et = t512("cum_et")
            excl_et = t512("excl_et")
            base = t512("base")
            cmp16 = t512("cmp16")
            nblk = sbt(sb_, "nblk", [128, 32], F32)
            pendb = sbt(sb_, "pendb", [128, 32], F32)
            pstart = sbt(sb_, "pstart", [128, 32], F32)
            ones32 = sbt(sb_, "ones32", [128, 32], F32)
            dd = sbt(sb_, "dd", [128, 2, T], F32)
            cmpb = sbt(sb_, "cmpb", [128, NB, 32], F32)
            ebf = sbt(sb_, "ebf", [128, NB], F32)

            gl = lg_all[:, :, 0:4]
            el4 = lg_all[:, :, 4:36].rearrange("p t (g e) -> p t g e", g=4)

            def bc(ap, axis, shape):
                return ap.unsqueeze(axis).to_broadcast(shape)

            V(lambda e: e.tensor_reduce(out=gmax[:], in_=gl, axis=AX.X, op=ALU.max), r=["lg_all"], w=["gmax"])
            V(lambda e: e.tensor_tensor(out=goh[:], in0=gl, in1=bc(gmax[:], 2, [128, T, 4]), op=ALU.is_equal), r=["lg_all", "gmax"], w=["goh"])
            V(lambda e: e.tensor_tensor(out=gd[:], in0=gl, in1=bc(gmax[:], 2, [128, T, 4]), op=ALU.subtract), r=["lg_all", "gmax"], w=["gd"])
            A(lambda e: e.activation(out=gd[:], in_=gd[:], func=AF.Exp), r=["gd"], w=["gd"])
            V(lambda e: e.tensor_reduce(out=gs[:], in_=gd[:], axis=AX.X, op=ALU.add), r=["gd"], w=["gs"])
            V(lambda e: e.reciprocal(out=gw[:], in_=gs[:]), r=["gs"], w=["gw"])
            tmp4 = tmp[:].rearrange("p (t g e) -> p t g e", t=T, g=4)
            V(lambda e: e.tensor_tensor(out=tmp4, in0=el4, in1=bc(goh[:], 3, [128, T, 4, 8]), op=ALU.mult), r=["lg_all", "goh"], w=["tmp"])
            V(lambda e: e.tensor_reduce(out=sel[:], in_=tmp[:].rearrange("p (t g e) -> p t e g", t=T, g=4), axis=AX.X, op=ALU.add), r=["tmp"], w=["sel"])
            V(lambda e: e.tensor_reduce(out=m1[:], in_=sel[:], axis=AX.X, op=ALU.max), r=["sel"], w=["m1"])
            V(lambda e: e.tensor_tensor(out=oh1[:], in0=sel[:], in1=bc(m1[:], 2, [128, T, 8]), op=ALU.is_equal), r=["sel", "m1"], w=["oh1"])
            V(lambda e: e.scalar_tensor_tensor(out=sel2[:], in0=oh1[:], scalar=-1e30, in1=sel[:], op0=ALU.mult, op1=ALU.add), r=["oh1", "sel"], w=["sel2"])
            V(lambda e: e.tensor_reduce(out=m2[:], in_=sel2[:], axis=AX.X, op=ALU.max), r=["sel2"], w=["m2"])
            V(lambda e: e.tensor_tensor(out=oh2[:], in0=sel2[:], in1=bc(m2[:], 2, [128, T, 8]), op=ALU.is_equal), r=["sel2", "m2"], w=["oh2"])
            V(lambda e: e.tensor_tensor(out=dm[:], in0=m2[:], in1=m1[:], op=ALU.subtract), r=["m1", "m2"], w=["dm"])
            A(lambda e: e.activation(out=dm[:], in_=dm[:], func=AF.Exp), r=["dm"], w=["dm"])
            V(lambda e: e.tensor_scalar(out=dm[:], in0=dm[:], scalar1=1.0, scalar2=None, op0=ALU.add), r=["dm"], w=["dm"])
            V(lambda e: e.reciprocal(out=dm[:], in_=dm[:]), r=["dm"], w=["dm"])
            V(lambda e: e.tensor_tensor(out=gates[:, 0, :], in0=gw[:], in1=dm[:], op=ALU.mult), r=["gw", "dm"], w=["gates0"])
            V(lambda e: e.tensor_tensor(out=gates[:, 1, :], in0=gw[:], in1=gates[:, 0, :], op=ALU.subtract), r=["gw", "gates0"], w=["gates1"])
            OH1v = OH1[:].rearrange("p (t g e) -> p t g e", t=T, g=4)
            OH2v = OH2[:].rearrange("p (t g e) -> p t g e", t=T, g=4)
            V(lambda e: e.tensor_tensor(out=OH1v, in0=bc(goh[:], 3, [128, T, 4, 8]), in1=bc(oh1[:], 2, [128, T, 4, 8]), op=ALU.mult), r=["goh", "oh1"], w=["OH1"])
            V(lambda e: e.tensor_tensor(out=OH2v, in0=bc(goh[:], 3, [128, T, 4, 8]), in1=bc(oh2[:], 2, [128, T, 4, 8]), op=ALU.mult), r=["goh", "oh2"], w=["OH2"])
            V(lambda e: e.tensor_tensor(out=OHb[:], in0=OH1[:], in1=OH2[:], op=ALU.add), r=["OH1", "OH2"], w=["OHb"])
            P(lambda e: e.matmul(pb[1][:, 0:512], lhsT=lstrict, rhs=OHb[:], start=True, stop=True), r=["cstb", "OHb"], w=["pb1"])
            P(lambda e: e.matmul(pb[2][:, 0:512], lhsT=onesb, rhs=OHb[:], start=True, stop=True), r=["cstb", "OHb"], w=["pb2"])
            tot3 = tot_et[:].rearrange("p (e t) -> p e t", e=32)
            V(lambda e: e.tensor_copy(out=tot3, in_=pb[2][:, 0:512].rearrange("p (t e) -> p e t", e=32)), r=["pb2"], w=["tot_et"])
            V(lambda e: e.memset(flags[:], 1.0), w=["flags"])
            V(lambda e: e.memset(flags[:].rearrange("p (e t) -> p e t", e=32)[:, :, 0:1], 0.0), r=["flags"], w=["flags"])
            V(lambda e: e.tensor_tensor_scan(out=cum_et[:], data0=flags[:], data1=tot_et[:], initial=0.0, op0=ALU.mult, op1=ALU.add),
              r=["flags", "tot_et"], w=["cum_et"])
            V(lambda e: e.tensor_tensor(out=excl_et[:], in0=cum_et[:], in1=tot_et[:], op=ALU.subtract), r=["cum_et", "tot_et"], w=["excl_et"])
            cnt_e = cum_et[:].rearrange("p (e t) -> p e t", e=32)[:, :, T - 1:T]
            c16 = cmp16[:].rearrange("p (e j) -> p e j", e=32)
            V(lambda e: e.tensor_tensor(out=c16, in0=cnt_e.to_broadcast([128, 32, 16]), in1=bc(rep[:, 44:60], 1, [128, 32, 16]), op=ALU.is_gt),
              r=["cum_et", "rep"], w=["cmp16"])
            V(lambda e: e.tensor_reduce(out=nblk[:], in_=c16, axis=AX.X, op=ALU.add), r=["cmp16"], w=["nblk"])
            V(lambda e: e.memset(ones32[:], 1.0), w=["ones32"])
            V(lambda e: e.tensor_tensor_scan(out=pendb[:], data0=ones32[:], data1=nblk[:], initial=0.0, op0=ALU.mult, op1=ALU.add),
              r=["ones32", "nblk"], w=["pendb"])
            V(lambda e: e.tensor_tensor(out=pstart[:], in0=pendb[:], in1=nblk[:], op=ALU.subtract), r=["pendb", "nblk"], w=["pstart"])
            V(lambda e: e.tensor_scalar(out=pstart[:], in0=pstart[:], scalar1=float(SBK), scalar2=None, op0=ALU.mult), r=["pstart"], w=["pstart"])
            V(lambda e: e.tensor_scalar(out=pendb[:], in0=pendb[:], scalar1=float(SBK), scalar2=None, op0=ALU.mult), r=["pendb"], w=["pendb"])
            base3 = base[:].rearrange("p (t e) -> p t e", e=32)
            V(lambda e: e.tensor_tensor(out=base3, in0=pb[1][:, 0:512].rearrange("p (t e) -> p t e", e=32),
                                        in1=excl_et[:].rearrange("p (e t) -> p t e", e=32), op=ALU.add), r=["pb1", "excl_et"], w=["base"])
            V(lambda e: e.tensor_tensor(out=base3, in0=base3, in1=bc(pstart[:], 1, [128, T, 32]), op=ALU.add), r=["base", "pstart"], w=["base"])
            for kk, OHk in enumerate((OH1, OH2)):
                V(lambda e: e.tensor_tensor(out=tmp[:], in0=OHk[:], in1=base[:], op=ALU.mult), r=[f"OH{kk + 1}", "base"], w=["tmp"])
                V(lambda e: e.tensor_reduce(out=dd[:, kk, :], in_=tmp[:].rearrange("p (t e) -> p t e", e=32), axis=AX.X, op=ALU.add), r=["tmp"], w=[f"dd{kk}"])
            V(lambda e: e.tensor_copy(out=dest_i[:], in_=dd[:]), r=["dd0", "dd1"], w=["dest_i"])
            V(lambda e: e.tensor_tensor(out=cmpb[:], in0=bc(pendb[:], 1, [128, NB, 32]), in1=bc(rep[:, 60:60 + NB], 2, [128, NB, 32]), op=ALU.is_le),
              r=["pendb", "rep"], w=["cmpb"])
            V(lambda e: e.tensor_reduce(out=ebf[:], in_=cmpb[:], axis=AX.X, op=ALU.add), r=["cmpb"], w=["ebf"])
            V(lambda e: e.tensor_scalar(out=ebf[:], in0=ebf[:], scalar1=31.0, scalar2=0.0, op0=ALU.min, op1=ALU.max), r=["ebf"], w=["ebf"])
            idxwf = sbt(sb_, "idxwf", [128, 2, NB], F32)
            validb = sbt(sb_, "validb", [128, NB], F32)
            V(lambda e: e.tensor_scalar(out=validb[:], in0=rep[:, 60:60 + NB], scalar1=pendb[:, 31:32], scalar2=None, op0=ALU.is_lt), r=["rep", "pendb"], w=["validb"])
            V(lambda e: e.tensor_scalar(out=ebf[:], in0=ebf[:], scalar1=-4000.0, scalar2=None, op0=ALU.add), r=["ebf"], w=["ebf"])
            V(lambda e: e.tensor_tensor(out=ebf[:], in0=ebf[:], in1=validb[:], op=ALU.mult), r=["ebf", "validb"], w=["ebf"])
            V(lambda e: e.tensor_scalar(out=ebf[:], in0=ebf[:], scalar1=4000.0, scalar2=None, op0=ALU.add), r=["ebf"], w=["ebf"])
            for a in range(2):
                V(lambda e: e.tensor_scalar(out=idxwf[:, a, :], in0=ebf[:], scalar1=256.0, scalar2=pcol[:, a:a + 1], op0=ALU.mult, op1=ALU.add),
                  r=["ebf", "pcol"], w=[f"idxwf{a}"])
            V(lambda e: e.tensor_copy(out=idxw[:], in_=idxwf[:]), r=["idxwf0", "idxwf1"], w=["idxw"])
            for pos in range(NWB):
                issue_weights(pos)
            hbt = [sbt(sb_, f"hbt{i}", [128, D], BF16) for i in range(4)]
            for t in range(T):
                k.dma("sp", f"ldhb{t % 4}", lambda e: e.dma_start(out=hbt[t % 4][:], in_=h1bs[t * 128:(t + 1) * 128, :]), reads=["h1bs"], writes=[f"hbt{t % 4}"])
                for kk in range(2):
                    k.dma("pool", f"xsc{t % 4}_{kk}", lambda e: e.indirect_dma_start(out=Xs, out_offset=bass.IndirectOffsetOnAxis(ap=dest_i[:, kk, t:t + 1], axis=0),
                                                                                     in_=hbt[t % 4][:], in_offset=None),
                          reads=[f"hbt{t % 4}", "dest_i"], writes=[f"Xs_sc{t % 4}_{kk}"])
            XS_ALL = [f"Xs_sc{i}_{kk}" for i in range(4) for kk in range(2)]

        with contextlib.ExitStack() as sd:
            NQ = NB * 2
            def rows_of(q):
                return (sb_order[q // 2] * 2 + q % 2) * 128

            def moe_s1(q):
                sbk, s3 = sb_order[q // 2], q % 3
                ws = (q // 2) % NWB
                if q % 2 == 0 and q // 2 >= NWB:
                    issue_weights(q // 2)
                for qq in ((0, 1, 2) if q == 0 else (q + 2,)):
                    if qq < NQ:
                        k.dma("sp", f"ldxb{qq % 3}", lambda e: e.dma_start(out=xb[qq % 3][:], in_=Xs[rows_of(qq):rows_of(qq) + 128, :]),
                              reads=XS_ALL, writes=[f"xb{qq % 3}"])
                pbt = pb[0][:].bitcast(BF16)
                for c in range(8):
                    P(lambda e: e.transpose(out=pbt[:, c * 128:(c + 1) * 128], in_=xb[s3][:, c * 128:(c + 1) * 128], identity=identb),
                      r=[f"xb{s3}", "cstb"], w=["pb0"])
                A(lambda e: e.copy(out=xbT[s3][:].rearrange("p c t -> p (c t)"), in_=pbt[:, 0:1024]), r=["pb0"], w=[f"xbT{s3}"])

            def moe_s2(q):
                ws, s3, s2 = (q // 2) % NWB, q % 3, q % 2
                pg, pgn = pb[1 + 2 * s2], f"pb{1 + 2 * s2}"
                pu, pun = pb[2 + 2 * s2], f"pb{2 + 2 * s2}"
                for c in range(8):
                    P(lambda e: e.matmul(pg[:, 0:512], lhsT=xbT[s3][:, c, :], rhs=wg[ws][:, c, :], start=(c == 0), stop=(c == 7)),
                      r=[f"xbT{s3}", f"wg{ws}_0", f"wg{ws}_1"], w=[pgn])
                for c in range(8):
                    P(lambda e: e.matmul(pu[:, 0:512], lhsT=xbT[s3][:, c, :], rhs=wu[ws][:, c, :], start=(c == 0), stop=(c == 7)),
                      r=[f"xbT{s3}", f"wu{ws}_0", f"wu{ws}_1"], w=[pun])
                A(lambda e: e.activation(out=sgm[:], in_=pg[:, 0:512], func=AF.Sigmoid), r=[pgn], w=["sgm"])
                V(lambda e: e.tensor_tensor(out=gsm[:], in0=sgm[:], in1=pg[:, 0:512], op=ALU.mult), r=["sgm", pgn], w=["gsm"])
                V(lambda e: e.tensor_tensor(out=ab[s2][:], in0=gsm[:], in1=pu[:, 0:512], op=ALU.mult), r=["gsm", pun], w=[f"ab{s2}"])

            def moe_s3(q):
                s2 = q % 2
                pbt5 = pb[5][:].bitcast(BF16)
                for c in range(4):
                    P(lambda e: e.transpose(out=pbt5[:, c * 128:(c + 1) * 128], in_=ab[s2][:, c * 128:(c + 1) * 128], identity=identb),
                      r=[f"ab{s2}", "cstb"], w=["pb5"])
                A(lambda e: e.copy(out=aT[s2][:].rearrange("p c t -> p (c t)"), in_=pbt5[:, 0:512]), r=["pb5"], w=[f"aT{s2}"])

            def moe_s4(q):
                ws, s2 = (q // 2) % NWB, q % 2
                for hv in range(2):
                    py, pyn = pb[6 + hv], f"pb{6 + hv}"
                    for c in range(4):
                        P(lambda e: e.matmul(py[:, 0:512], lhsT=aT[s2][:, c, :], rhs=wd[ws][:, c, hv * 512:(hv + 1) * 512], start=(c == 0), stop=(c == 3)),
                          r=[f"aT{s2}", f"wd{ws}_0", f"wd{ws}_1"], w=[pyn])
                    A(lambda e: e.copy(out=yb[s2][:, hv * 512:(hv + 1) * 512], in_=py[:, 0:512]), r=[pyn], w=[f"yb{s2}{'ab'[hv]}"])
                k.dma("act", f"yst{s2}", lambda e: e.dma_start(out=Ys[rows_of(q):rows_of(q) + 128, :], in_=yb[s2][:]), reads=[f"yb{s2}a", f"yb{s2}b"], writes=[f"Ys{s2}"])

            for i in range(NQ + 3):
                if i < NQ:
                    moe_s1(i)
                if 0 <= i - 1 < NQ:
                    moe_s2(i - 1)
                if 0 <= i - 2 < NQ:
                    moe_s3(i - 2)
                if 0 <= i - 3 < NQ:
                    moe_s4(i - 3)

            def comb_loads(t):
                s = t % 2
                k.dma("pool", f"g0_{s}", lambda e: e.indirect_dma_start(out=y0[s][:], out_offset=None, in_=Ys,
                                                                        in_offset=bass.IndirectOffsetOnAxis(ap=dest_i[:, 0, t:t + 1], axis=0)),
                      reads=["Ys0", "Ys1", "dest_i"], writes=[f"y0_{s}"])
                k.dma("pool", f"g1_{s}", lambda e: e.indirect_dma_start(out=y1[s][:], out_offset=None, in_=Ys,
                                                                        in_offset=bass.IndirectOffsetOnAxis(ap=dest_i[:, 1, t:t + 1], axis=0)),
                      reads=["Ys0", "Ys1", "dest_i"], writes=[f"y1_{s}"])
                k.dma("sp", f"ldh{s}", lambda e: e.dma_start(out=hr[s][:], in_=h1s[t * 128:(t + 1) * 128, :]), reads=["h1s"], writes=[f"hr{s}"])

            def comb_s1(t):
                s = t % 2
                A(lambda e: e.activation(out=zcb[t % 3][:], in_=y0[s][:], func=AF.Copy, scale=gates[:, 0, t:t + 1]), r=[f"y0_{s}", "gates0"], w=[f"zc{t % 3}"])
                V(lambda e: e.scalar_tensor_tensor(out=zcb[t % 3][:], in0=y1[s][:], scalar=gates[:, 1, t:t + 1], in1=zcb[t % 3][:], op0=ALU.mult, op1=ALU.add),
                  r=[f"y1_{s}", "gates1", f"zc{t % 3}"], w=[f"zc{t % 3}"])
                V(lambda e: e.scalar_tensor_tensor(out=zcb[t % 3][:], in0=hr[s][:], scalar=ALPHA, in1=zcb[t % 3][:], op0=ALU.mult, op1=ALU.add), r=[f"hr{s}", f"zc{t % 3}"], w=[f"zc{t % 3}"])
                for hv in range(2):
                    V(lambda e: e.bn_stats(out=stats2[s][:, hv, :], in_=zcb[t % 3][:, hv * 512:(hv + 1) * 512]), r=[f"zc{t % 3}"], w=[f"st2{hv}_{s}"])
                V(lambda e: e.bn_aggr(out=mv2[s][:], in_=stats2[s][:].rearrange("p a s -> p (a s)")), r=[f"st20_{s}", f"st21_{s}"], w=[f"mv2_{s}"])

            def comb_s2(t):
                s = t % 2
                A(lambda e: e.activation(out=sm2[s][:, 0:1], in_=mv2[s][:, 1:2], func=AF.Ln, bias=EPS, scale=1.0), r=[f"mv2_{s}"], w=[f"sm2a{s}"])
                A(lambda e: e.activation(out=sm2[s][:, 1:2], in_=sm2[s][:, 0:1], func=AF.Exp, scale=-0.5), r=[f"sm2a{s}"], w=[f"sm2b{s}"])
                V(lambda e: e.scalar_tensor_tensor(out=sm2[s][:, 2:3], in0=mv2[s][:, 0:1], scalar=-1.0, in1=sm2[s][:, 1:2], op0=ALU.mult, op1=ALU.mult),
                  r=[f"mv2_{s}", f"sm2b{s}"], w=[f"sm2c{s}"])
                A(lambda e: e.activation(out=zn[s][:], in_=zcb[t % 3][:], func=AF.Identity, bias=sm2[s][:, 2:3], scale=sm2[s][:, 1:2]),
                  r=[f"zc{t % 3}", f"sm2b{s}", f"sm2c{s}"], w=[f"zn{s}"])

            def comb_s3(t):
                s = t % 2
                GP(lambda e: e.tensor_tensor(out=zn[s][:], in0=zn[s][:], in1=lnp2[:, 0, :], op=ALU.mult), r=[f"zn{s}", "lnp2"], w=[f"zn{s}"])
                V(lambda e: e.tensor_tensor(out=oc[s][:], in0=zn[s][:], in1=lnp2[:, 1, :], op=ALU.add), r=[f"zn{s}", "lnp2"], w=[f"oc{s}"])
                k.dma("sp", f"outst{s}", lambda e: e.dma_start(out=out_d[t * 128:(t + 1) * 128, :], in_=oc[s][:]), reads=[f"oc{s}"], writes=["out"])

            comb_loads(0)
            comb_loads(1)
            comb_s1(0)
            for t in range(NT_MAIN):
                if t + 1 < NT_MAIN:
                    comb_s1(t + 1)
                if t + 2 < NT_MAIN:
                    comb_loads(t + 2)
                comb_s2(t)
                comb_s3(t)
            k.barrier()
    return nc


def _host_constants():
    kk = np.arange(128, dtype=np.float32)[:, None]
    qq = np.arange(128, dtype=np.float32)[None, :]
    slopes = np.exp2(-8.0 * np.arange(1, 9, dtype=np.float32) / 8.0).astype(np.float32)
    cur = np.zeros((128, 8, 128), np.float32)
    prev = np.zeros((128, 8, 128), np.float32)
    for h in range(8):
        cur[:, h, :] = (kk <= qq) * np.exp(-slopes[h] * (qq - kk))
        prev[:, h, :] = (kk > qq) * np.exp(-slopes[h] * (qq - kk + 128.0))
    ident = np.eye(128, dtype=np.float32)
    lstrict = (kk < qq).astype(np.float32)
    ones = np.ones((128, 128), np.float32)
    cf = np.concatenate([ident, ident, lstrict, ones], axis=1)
    return cur, prev, cf


def kernel(x, meta_tokens, w_in, conv_w, conv_b, lru_wa, lru_ba, lru_wx, lru_bx, lru_lambda,
           attn_sinks, g_attn, g_lru, w_out, ln1_g, ln1_b, w_group, b_group, w_router,
           b_router, w_gate, w_up, w_down, ln2_g, ln2_b, _stage=3):
    f32 = np.float32
    x = np.asarray(x, f32)
    B = x.shape[0]
    cur, prev, cf = _host_constants()
    w_in0 = np.asarray(w_in, f32)[0]
    qcols = w_in0[:, 0:512].reshape(D, 2, 4, 64).transpose(0, 2, 1, 3).reshape(D, 512)
    w_in_p = np.ascontiguousarray(np.concatenate([qcols, w_in0[:, 512:]], axis=1))
    wbd = np.zeros((128, 2, 4, 128), f32)
    for wi, wsrc in enumerate((np.asarray(lru_wa, f32)[0], np.asarray(lru_wx, f32)[0])):
        for blk in range(8):
            j, half = blk // 2, blk % 2
            wbd[half * 64:(half + 1) * 64, wi, j, half * 64:(half + 1) * 64] = wsrc[blk]
    wbd = wbd.reshape(128, -1)

    def pc(v):
        return np.asarray(v, f32).reshape(-1, 128).T

    cw = np.asarray(conv_w, f32)[0]
    cwp = np.stack([pc(cw[kk]) for kk in range(4)], axis=2).reshape(128, 16)
    gcat = np.concatenate([np.asarray(g_attn, f32)[0], np.asarray(g_lru, f32)[0]])
    pp = np.concatenate([cwp, pc(np.asarray(conv_b, f32)[0]), pc(np.asarray(lru_ba, f32)[0].reshape(-1)),
                         pc(np.asarray(lru_bx, f32)[0].reshape(-1)), pc(np.asarray(lru_lambda, f32)[0]), pc(gcat)], axis=1)
    pp = np.ascontiguousarray(pp, f32)
    biases = np.concatenate([np.asarray(b_group, f32)[0], np.asarray(b_router, f32)[0].reshape(-1)])
    rep_row = np.concatenate([np.asarray(attn_sinks, f32)[0], biases, float(SBK) * np.arange(16, dtype=f32), float(SBK) * np.arange(64, dtype=f32)])
    rep = np.ascontiguousarray(np.broadcast_to(rep_row[None, :], (128, rep_row.size)), f32)
    ln_row = np.concatenate([np.asarray(a, f32)[0] for a in (ln1_g, ln1_b, ln2_g, ln2_b)])
    ln = np.ascontiguousarray(np.broadcast_to(ln_row[None, :], (128, ln_row.size)), f32)
    w_r = np.ascontiguousarray(np.concatenate([np.asarray(w_group, f32)[0],
                                               np.asarray(w_router, f32)[0].transpose(1, 0, 2).reshape(D, 32)], axis=1), f32)
    w_out0 = np.ascontiguousarray(np.asarray(w_out, f32)[0])
    def relayout(w):
        e_, kdim, fdim = w.shape
        r = w.reshape(e_, kdim // 128, 128, fdim).transpose(0, 2, 1, 3)
        return np.ascontiguousarray(r).reshape(e_ * 256, 2048)

    wg0 = relayout(np.asarray(w_gate, f32)[0])
    wu0 = relayout(np.asarray(w_up, f32)[0])
    wd0 = relayout(np.asarray(w_down, f32)[0])
    pcolv = np.ascontiguousarray(np.stack([2.0 * np.arange(128), 2.0 * np.arange(128) + 1.0], axis=1), f32)
    meta = np.asarray(meta_tokens, f32)

    in_maps = []
    for b in range(B):
        seq = np.concatenate([np.zeros((112, D), f32), meta, x[b]], axis=0)
        for hf in range(2):
            if hf == 1:
                xs = seq
                nfake = 112
                xmain = x[b, 2048:4096]
            else:
                xs = np.concatenate([np.zeros((16 * 128, D), f32), seq[0:17 * 128]], axis=0)
                nfake = 16 * 128 + 112
                xmain = x[b, 0:2048]
            tm = np.ones((NT_PRE * 128,), f32)
            tm[:nfake] = 0.0
            prev0 = prev.copy()
            if hf == 0:
                prev0[0:112] = 0.0
            masks = np.stack([cur, prev, prev0], axis=1).reshape(128, -1)
            in_maps.append({
                "xsT": np.ascontiguousarray(xs.T), "xm": np.ascontiguousarray(xmain),
                "tmask": np.ascontiguousarray(np.broadcast_to(tm[None, :], (128, tm.size))),
                "masks": np.ascontiguousarray(masks), "w_in": w_in_p, "w_out": w_out0, "wbd": wbd, "pp": pp, "rep": rep,
                "ln": ln, "w_r": w_r, "cf": cf, "w_gate": wg0, "w_up": wu0, "w_down": wd0, "pcol": pcolv,
            })
    nc = build_program(stage=_stage)
    res = run_bass_kernel_spmd(nc, in_maps, core_ids=list(range(2 * B)))
    out = np.empty((B, 4096, D), f32)
    for b in range(B):
        for hf in range(2):
            out[b, hf * 2048:(hf + 1) * 2048] = res.results[b * 2 + hf]["out"]
    return out
```

```python
import contextlib
import numpy as np
import concourse.bass as bass
import concourse.mybir as mybir
from concourse.bass_utils import run_bass_kernel_spmd

F32 = mybir.dt.float32
BF16 = mybir.dt.bfloat16
I32 = mybir.dt.int32
AF = mybir.ActivationFunctionType
ALU = mybir.AluOpType
AX = mybir.AxisListType

D = 1024
NT_MAIN = 16
NT_PRE = 17
NTILES = NT_PRE + NT_MAIN
NB = 48
SBK = 256
ALPHA = 2.0 ** 0.25
EPS = 1e-5
QCOL, KCOL, VCOL, XRCOL, YRCOL = 0, 512, 640, 768, 1280
GELU_C = 1.5957691216057308


class KB:
    def __init__(self, nc, stack):
        self.nc = nc
        self.stack = stack
        self.E = dict(pe=nc.tensor, dve=nc.vector, act=nc.scalar, pool=nc.gpsimd, sp=nc.sync)
        self.sems = {}
        self.cnt = {}
        for e in ("pe", "dve", "act", "pool"):
            self.sems[e] = stack.enter_context(nc.semaphore("sem_" + e))
            self.cnt[e] = 0
        self.seen = {e: {} for e in self.E}
        self.lastw = {}
        self.reads = {}

    def dma_sem(self, name):
        if name not in self.sems:
            self.sems[name] = self.stack.enter_context(self.nc.semaphore("sem_" + name))
            self.cnt[name] = 0
        return name

    def _need(self, reads, writes):
        need = {}

        def add(k, v):
            if v > need.get(k, 0):
                need[k] = v

        for r in reads:
            lw = self.lastw.get(r)
            if lw:
                add(*lw)
        for w in writes:
            lw = self.lastw.get(w)
            if lw:
                add(*lw)
            for k, v in self.reads.get(w, {}).items():
                add(k, v)
        return need

    def _emit_waits(self, eng, need):
        for k, v in need.items():
            if k == eng and eng == "pe":
                continue
            if self.seen[eng].get(k, 0) >= v:
                continue
            self.E[eng].wait_ge(self.sems[k], v)
            self.seen[eng][k] = v

    def sync_reads(self, eng, reads):
        self._emit_waits(eng, self._need(reads, []))

    def _record(self, key, val, reads, writes):
        for r in reads:
            d = self.reads.setdefault(r, {})
            if val > d.get(key, 0):
                d[key] = val
        for w in writes:
            self.lastw[w] = (key, val)
            self.reads[w] = {}

    def op(self, eng, fn, reads=(), writes=()):
        self._emit_waits(eng, self._need(reads, writes))
        inst = fn(self.E[eng])
        self.cnt[eng] += 1
        inst.then_inc(self.sems[eng], 1)
        self._record(eng, self.cnt[eng], reads, writes)
        return inst

    def dma(self, eng, sem, fn, reads=(), writes=()):
        self.dma_sem(sem)
        self._emit_waits(eng, self._need(reads, writes))
        inst = fn(self.E[eng])
        self.cnt[sem] += 16
        inst.then_inc(self.sems[sem], 16)
        self._record(sem, self.cnt[sem], reads, writes)
        return inst

    def seal(self, sems):
        for r, (key, _) in list(self.lastw.items()):
            if key in sems:
                self.lastw[r] = (key, self.cnt[key])

    def barrier(self):
        for eng in self.E:
            for k2, v in self.cnt.items():
                if v and self.seen[eng].get(k2, 0) < v:
                    self.E[eng].wait_ge(self.sems[k2], v)
                    self.seen[eng][k2] = v
        self.lastw = {}
        self.reads = {}


def build_program(stage=3):
    nc = bass.Bass("TRN2", target_bir_lowering=False)

    def din(name, shape, dt=F32):
        return nc.dram_tensor(name, list(shape), dt, kind="ExternalInput").ap()

    xsT = din("xsT", [D, NTILES * 128])
    xm = din("xm", [NT_MAIN * 128, D])
    tmask_d = din("tmask", [128, NT_PRE * 128])
    masks_d = din("masks", [128, 3 * 8 * 128])
    w_in_d = din("w_in", [D, 1792])
    w_out_d = din("w_out", [D, D])
    wbd_d = din("wbd", [128, 2 * 4 * 128])
    pp_d = din("pp", [128, 40])
    rep_d = din("rep", [128, 124])
    ln_d = din("ln", [128, 4 * D])
    w_r_d = din("w_r", [D, 36])
    cf_d = din("cf", [128, 4 * 128])
    w_gate_d = din("w_gate", [32 * 256, 2048])
    w_up_d = din("w_up", [32 * 256, 2048])
    w_down_d = din("w_down", [32 * 256, 2048])
    pcol_d = din("pcol", [128, 2])
    out_d = nc.dram_tensor("out", [NT_MAIN * 128, D], F32, kind="ExternalOutput").ap()
    Xs = nc.dram_tensor("Xs", [NB * SBK, D], BF16, kind="Internal").ap()
    Ys = nc.dram_tensor("Ys", [NB * SBK, D], F32, kind="Internal").ap()
    h1s = nc.dram_tensor("h1s", [NT_MAIN * 128, D], F32, kind="Internal").ap()
    h1bs = nc.dram_tensor("h1bs", [NT_MAIN * 128, D], BF16, kind="Internal").ap()

    with contextlib.ExitStack() as st0:
        k = KB(nc, st0)

        def sbt(stack, name, shape, dt):
            return stack.enter_context(nc.sbuf_tensor("s_" + name, list(shape), dt))

        pb = [st0.enter_context(nc.psum_tensor(f"pb{i}", [128, 512], F32)) for i in range(8)]

        def V(fn, r=(), w=()):
            return k.op("dve", fn, r, w)

        def A(fn, r=(), w=()):
            return k.op("act", fn, r, w)

        def P(fn, r=(), w=()):
            return k.op("pe", fn, r, w)

        def GP(fn, r=(), w=()):
            return k.op("pool", fn, r, w)

        lg_all = sbt(st0, "lg_all", [128, NT_MAIN, 36], F32)
        rep = sbt(st0, "rep", [128, 124], F32)
        cstb = sbt(st0, "cstb", [128, 3, 128], BF16)
        identf = sbt(st0, "identf", [128, 128], F32)
        identb = cstb[:, 0, :]
        lstrict = cstb[:, 1, :]
        onesb = cstb[:, 2, :]

        k.dma("sp", "c0", lambda e: e.dma_start(out=rep[:], in_=rep_d), writes=["rep"])
        k.dma("sp", "c0", lambda e: e.dma_start(out=identf[:], in_=cf_d[:, 0:128]), writes=["identf"])
        k.dma("pool", "c1", lambda e: e.dma_start(out=cstb[:].rearrange("p a d -> p (a d)"), in_=cf_d[:, 128:512]), writes=["cstb"])

        with contextlib.ExitStack() as sa:
            win = sbt(sa, "win", [128, 8, 1792], BF16)
            wo = sbt(sa, "wo", [128, 8, D], BF16)
            lnp = sbt(sa, "lnp", [128, 2, D], F32)
            h1bt = [sbt(sa, f"h1bt{i}", [128, D], BF16) for i in range(2)]
            wbd = sbt(sa, "wbd", [128, 2, 4, 128], BF16)
            ppt = sbt(sa, "ppt", [128, 40], F32)
            sc = sbt(sa, "sc", [128, 4], F32)
            nba = sbt(sa, "nba", [128, 8], F32)
            esink = sbt(sa, "esink", [128, 8], F32)
            wr = sbt(sa, "wr", [128, 8, 36], F32)
            msk = sbt(sa, "msk", [128, 3, 8, 128], BF16)
            zt = sbt(sa, "zt", [128, D], BF16)
            xT = [sbt(sa, f"xT{i}", [128, 8, 512], BF16) for i in range(2)]
            xres = [sbt(sa, f"xres{i}", [128, D], F32) for i in range(2)]
            tmask = xres[0]
            qT = sbt(sa, "qT", [128, 4, 512], BF16)
            kT = sbt(sa, "kT", [128, NT_MAIN + 1, 128], BF16)
            vbuf = sbt(sa, "vbuf", [128, NT_MAIN + 1, 2, 65], BF16)
            xr4 = sbt(sa, "xr4", [128, 4, 3 + 512], F32)
            xrh = sbt(sa, "xrh", [128, 4, 3], F32)
            hlast = sbt(sa, "hlast", [128, 4], F32)
            xc4 = sbt(sa, "xc4", [128, 4, 512], F32)
            xcb4 = sbt(sa, "xcb4", [128, 4, 512], BF16)
            ra4 = sbt(sa, "ra4", [128, 4, 512], F32)
            ts4 = sbt(sa, "ts4", [128, 4, 512], F32)
            iu4 = sbt(sa, "iu4", [128, 4, 512], F32)
            lruf = sbt(sa, "lruf", [128, 4, 512], F32)
            sqb = sbt(sa, "sqb", [128, 4, 512], BF16)
            slbc = sbt(sa, "slbc", [128, 512], F32)
            lruT = sbt(sa, "lruT", [128, 4, 512], BF16)
            ebuf = [sbt(sa, f"ebuf{i}", [128, 512], F32) for i in range(2)]
            pT = [sbt(sa, f"pT{i}", [128, 2, 2, 4, 128], BF16) for i in range(1)]
            zz = sbt(sa, "zz", [128, 8], F32)
            rz = sbt(sa, "rz", [128, 8], F32)
            attnf = sbt(sa, "attnf", [128, 512], F32)
            junk = sbt(sa, "junk", [128, 512], BF16)
            sml = sbt(sa, "sml", [128, 16], F32)
            attnb = sbt(sa, "attnb", [128, 512], BF16)
            attnT = [sbt(sa, f"attnT{i}", [128, 4, 128], BF16) for i in range(4)]
            zbs = [sbt(sa, f"zb{i}", [128, D], F32) for i in range(2)]
            zb = zbs[0]
            h1fs = [sbt(sa, f"h1f{i}", [128, D], F32) for i in range(2)]
            h1f = h1fs[0]
            h1T = sbt(sa, "h1T", [128, 4, 128], F32)
            stats = sbt(sa, "stats", [128, 2, 6], F32)
            mv = sbt(sa, "mv", [128, 2], F32)

            k.dma("pool", "c1", lambda e: e.dma_start(out=win[:], in_=w_in_d.rearrange("(c p) n -> p c n", p=128)), writes=["win"])
            k.dma("pool", "c1", lambda e: e.dma_start(out=wbd[:].rearrange("p a c d -> p (a c d)"), in_=wbd_d), writes=["wbd"])
            k.dma("sp", "c0", lambda e: e.dma_start(out=ppt[:], in_=pp_d), writes=["ppt"])
            k.dma("sp", "c0", lambda e: e.dma_start(out=wr[:], in_=w_r_d.rearrange("(c p) n -> p c n", p=128)), writes=["wr"])
            k.dma("pool", "c1", lambda e: e.dma_start(out=msk[:].rearrange("p a h q -> p (a h q)"), in_=masks_d, max_dma_last_dim=4096), writes=["msk"])
            k.dma("sp", "c0", lambda e: e.dma_start(out=lnp[:].rearrange("p a d -> p (a d)"), in_=ln_d[:, 0:2 * D]), writes=["lnp"])
            k.seal(("c0", "c1"))
            wstage = [zb, h1f]
            for c in range(8):
                ws, wsn = wstage[c % 2], (["zb0", "zb1"], ["h1f"])[c % 2]
                k.dma("sp", f"wst{c%2}", lambda e: e.dma_start(out=ws[:], in_=w_out_d[c * 128:(c + 1) * 128, :]), writes=wsn)
                V(lambda e: e.tensor_scalar(out=wo[:, c, :], in0=ws[:], scalar1=ppt[:, 32 + c:33 + c], scalar2=None, op0=ALU.mult),
                  r=wsn + ["ppt"], w=["wo"])
            A(lambda e: e.activation(out=sc[:], in_=ppt[:, 28:32], func=AF.Exp, scale=-1.0), r=["ppt"], w=["sc"])
            A(lambda e: e.activation(out=sc[:], in_=sc[:], func=AF.Ln, bias=1.0, scale=1.0), r=["sc"], w=["sc"])
            V(lambda e: e.tensor_scalar(out=sc[:], in0=sc[:], scalar1=-8.0, scalar2=None, op0=ALU.mult), r=["sc"], w=["sc"])
            A(lambda e: e.activation(out=esink[:], in_=rep[:, 0:8], func=AF.Exp), r=["rep"], w=["esink"])
            V(lambda e: e.memset(zt[:], 0.0), w=["zt"])
            V(lambda e: e.memset(vbuf[:], 1.0), w=["vbuf"])
            V(lambda e: e.memset(xrh[:], 0.0), w=["xrh"])
            V(lambda e: e.memset(hlast[:], 0.0), w=["hlast0", "hlast2"])

            xsT_v = xsT.rearrange("(c p) t -> p c t", p=128)
            pcount = [0]

            def next_pb12():
                pcount[0] += 1
                i = 1 + (pcount[0] % 2)
                return pb[i], f"pb{i}"

            def proj_fm(col0, N, xt, xtn):
                ps, psn = next_pb12()
                for c in range(8):
                    P(lambda e: e.matmul(ps[:, 0:N], lhsT=win[:, c, col0:col0 + 128], rhs=xt[:, c, 0:N], start=(c == 0), stop=(c == 7)),
                      r=["win", xtn], w=[psn])
                return ps, psn

            gi = [0]
            sc_cnt = [0]

            pb_rot = [1, 2, 3, 4]

            def next_pb14():
                pcount[0] += 1
                i = pb_rot[pcount[0] % len(pb_rot)]
                return pb[i], f"pb{i}"

            def proj4(col0, N, xt, xtn):
                ps, psn = next_pb14()
                for c in range(8):
                    P(lambda e: e.matmul(ps[:, 0:N], lhsT=win[:, c, col0:col0 + 128], rhs=xt[:, c, 0:N], start=(c == 0), stop=(c == 7)),
                      r=["win", xtn], w=[psn])
                return ps, psn

            XR = [f"xr4_{j}" for j in range(4)]
            XC = [f"xc4_{j}" for j in range(4)]
            RA = [f"ra4_{j}" for j in range(4)]
            IU = [f"iu4_{j}" for j in range(4)]
            TS = [f"ts4_{j}" for j in range(4)]

            GROUPS = [(g0 * 4, 4, "prefix") for g0 in range(4)] + [(16, 1, "halo")] + [(NT_PRE + g0 * 4, 4, "main") for g0 in range(4)]

            def load_xT(gidx):
                t0_, G_, _ = GROUPS[gidx]
                sl = gidx % 2
                k.dma("pool", f"ldx{sl}", lambda e: e.dma_start(out=xT[sl][:, :, 0:128 * G_], in_=xsT_v[:, :, t0_ * 128:t0_ * 128 + 128 * G_]),
                      writes=[f"xT{sl}"])

            def do_group(t0, G, kind):
                N = 128 * G
                slot = gi[0] % 2
                gi[0] += 1
                xt, xtn = xT[slot], f"xT{slot}"
                if gi[0] == 1:
                    load_xT(0)
                if gi[0] < len(GROUPS):
                    load_xT(gi[0])
                main = kind == "main"
                if main:
                    nz = NB * SBK // 128
                    gq = (t0 - NT_PRE) // 4
                    for i in range(gq * nz // 4, (gq + 1) * nz // 4):
                        k.dma("sp", "xz", lambda e: e.dma_start(out=Xs[i * 128:(i + 1) * 128, :], in_=zt[:]),
                              reads=["zt"], writes=["Xs"])
                use_mask = (kind == "halo") or (kind == "prefix" and t0 == 0)
                if use_mask:
                    k.dma("sp", "ldtm", lambda e: e.dma_start(out=tmask[:, 0:N], in_=tmask_d[:, t0 * 128:t0 * 128 + N]), writes=["xres0"])
                pb_rot[:] = [1, 2, 7] if main else [1, 2, 3, 4]

                def p0():
                    V(lambda e: e.tensor_copy(out=xr4[:, :, 0:3], in_=xrh[:]), r=["xrh", "xrh0", "xrh2"], w=["xr4h"])
                    for j in range(4):
                        ps, psn = proj4(XRCOL + j * 128, N, xt, xtn)
                        A(lambda e: e.copy(out=xr4[:, j, 3:3 + N], in_=ps[:, 0:N]), r=[psn], w=[XR[j]])
                    if main:
                        for g in range(4):
                            ps, psn = proj4(QCOL + g * 128, N, xt, xtn)
                            A(lambda e: e.copy(out=qT[:, g, 0:N], in_=ps[:, 0:N]), r=[psn], w=["qT"])
                    if kind in ("main", "halo"):
                        ps, psn = proj4(KCOL, N, xt, xtn)
                        ks0 = t0 - (NT_PRE - 1)
                        A(lambda e: e.copy(out=kT[:, ks0:ks0 + G, :], in_=ps[:, 0:N].rearrange("p (g t) -> p g t", g=G)),
                          r=[psn], w=[f"kT{ks0 + j}" for j in range(G)])
                        for tl in range(G):
                            ps, psn = next_pb14()
                            for c in range(8):
                                P(lambda e: e.matmul(ps[:, 0:128], lhsT=xt[:, c, tl * 128:(tl + 1) * 128], rhs=win[:, c, VCOL:VCOL + 128],
                                                     start=(c == 0), stop=(c == 7)), r=["win", xtn], w=[psn])
                            A(lambda e: e.copy(out=vbuf[:, ks0 + tl, :, 0:64], in_=ps[:, 0:128].rearrange("p (a d) -> p a d", a=2)),
                              r=[psn], w=[f"v{ks0 + tl}"])

                def p1(j0, nj):
                    J = slice(j0, j0 + nj)
                    for j in range(j0, j0 + nj):
                        V(lambda e: e.tensor_scalar(out=xc4[:, j, 0:N], in0=xr4[:, j, 0:N], scalar1=ppt[:, j * 4:j * 4 + 1],
                                                    scalar2=ppt[:, 16 + j:17 + j], op0=ALU.mult, op1=ALU.add), r=[XR[j], "xr4h", "ppt"], w=[XC[j]])
                        for tap in (1, 2, 3):
                            V(lambda e: e.scalar_tensor_tensor(out=xc4[:, j, 0:N], in0=xr4[:, j, tap:tap + N], scalar=ppt[:, j * 4 + tap:j * 4 + tap + 1],
                                                               in1=xc4[:, j, 0:N], op0=ALU.mult, op1=ALU.add), r=[XR[j], "xr4h", XC[j], "ppt"], w=[XC[j]])
                    V(lambda e: e.tensor_copy(out=xrh[:, J, :], in_=xr4[:, J, N:N + 3]), r=XR[J] + ["xr4h"], w=[f"xrh{j0}"])
                    A(lambda e: e.copy(out=xcb4[:, J, 0:N], in_=xc4[:, J, 0:N]), r=XC[J], w=[f"xcb4_{j0}"])

                def p2(j0, nj):
                    for j in range(j0, j0 + nj):
                        psr, psrn = next_pb14()
                        P(lambda e: e.matmul(psr[:, 0:N], lhsT=wbd[:, 0, j, :], rhs=xcb4[:, j, 0:N], start=True, stop=True), r=["wbd", f"xcb4_{j0}"], w=[psrn])
                        A(lambda e: e.activation(out=ra4[:, j, 0:N], in_=psr[:, 0:N], func=AF.Sigmoid, bias=ppt[:, 20 + j:21 + j]), r=[psrn, "ppt"], w=[RA[j]])
                        psi, psin = next_pb14()
                        P(lambda e: e.matmul(psi[:, 0:N], lhsT=wbd[:, 1, j, :], rhs=xcb4[:, j, 0:N], start=True, stop=True), r=["wbd", f"xcb4_{j0}"], w=[psin])
                        A(lambda e: e.activation(out=iu4[:, j, 0:N], in_=psi[:, 0:N], func=AF.Sigmoid, bias=ppt[:, 24 + j:25 + j]), r=[psin, "ppt"], w=[IU[j]])
                    for j in range(j0, j0 + nj):
                        A(lambda e: e.activation(out=ra4[:, j, 0:N], in_=ra4[:, j, 0:N], func=AF.Exp, scale=sc[:, j:j + 1]), r=[RA[j], "sc"], w=[RA[j]])

                def p3(j0, nj):
                    J = slice(j0, j0 + nj)
                    V(lambda e: e.scalar_tensor_tensor(out=ts4[:, J, 0:N], in0=ra4[:, J, 0:N], scalar=0.99999994, in1=ra4[:, J, 0:N], op0=ALU.min, op1=ALU.mult),
                      r=RA[J], w=TS[J])
                    A(lambda e: e.activation(out=ts4[:, J, 0:N], in_=ts4[:, J, 0:N], func=AF.Ln, bias=1.0, scale=-1.0), r=TS[J], w=TS[J])
                    A(lambda e: e.activation(out=ts4[:, J, 0:N], in_=ts4[:, J, 0:N], func=AF.Exp, scale=0.5), r=TS[J], w=TS[J])
                    V(lambda e: e.tensor_tensor(out=iu4[:, J, 0:N], in0=iu4[:, J, 0:N], in1=xc4[:, J, 0:N], op=ALU.mult), r=IU[J] + XC[J], w=IU[J])
                    V(lambda e: e.tensor_tensor(out=iu4[:, J, 0:N], in0=iu4[:, J, 0:N], in1=ts4[:, J, 0:N], op=ALU.mult), r=IU[J] + TS[J], w=IU[J])
                    if use_mask:
                        tmb = tmask[:, 0:N].unsqueeze(1).to_broadcast([128, nj, N])
                        V(lambda e: e.tensor_tensor(out=ra4[:, J, 0:N], in0=ra4[:, J, 0:N], in1=tmb, op=ALU.mult), r=RA[J] + ["xres0"], w=RA[J])
                        V(lambda e: e.tensor_tensor(out=iu4[:, J, 0:N], in0=iu4[:, J, 0:N], in1=tmb, op=ALU.mult), r=IU[J] + ["xres0"], w=IU[J])

                def p4(j0, nj):
                    J = slice(j0, j0 + nj)
                    for j in range(j0, j0 + nj):
                        V(lambda e: e.tensor_tensor_scan(out=ts4[:, j, 0:N], data0=ra4[:, j, 0:N], data1=iu4[:, j, 0:N], initial=hlast[:, j:j + 1],
                                                         op0=ALU.mult, op1=ALU.add), r=[RA[j], IU[j], TS[j], f"hlast{j0}"], w=[TS[j]])
                    V(lambda e: e.tensor_copy(out=hlast[:, J].unsqueeze(2), in_=ts4[:, J, N - 1:N]), r=TS[J], w=[f"hlast{j0}"])
                    if not main:
                        return
                    for j in range(j0, j0 + nj):
                        psy, psyn = proj4(YRCOL + j * 128, N, xt, xtn)
                        A(lambda e: e.copy(out=xr4[:, j, 3:3 + N], in_=psy[:, 0:N]), r=[psyn], w=[XR[j]])
                    yy = xr4[:, J, 3:3 + N]
                    t2 = xc4[:, J, 0:N]
                    V(lambda e: e.tensor_tensor(out=t2, in0=yy, in1=yy, op=ALU.mult), r=XR[J], w=XC[J])
                    V(lambda e: e.tensor_scalar(out=t2, in0=t2, scalar1=0.044715, scalar2=1.0, op0=ALU.mult, op1=ALU.add), r=XC[J], w=XC[J])
                    V(lambda e: e.tensor_tensor(out=t2, in0=t2, in1=yy, op=ALU.mult), r=XC[J] + XR[J], w=XC[J])
                    A(lambda e: e.activation(out=t2, in_=t2, func=AF.Sigmoid, scale=GELU_C), r=XC[J], w=XC[J])
                    V(lambda e: e.tensor_tensor(out=t2, in0=t2, in1=yy, op=ALU.mult), r=XC[J] + XR[J], w=XC[J])
                    V(lambda e: e.tensor_tensor(out=lruf[:, J, 0:N], in0=t2, in1=ts4[:, J, 0:N], op=ALU.mult), r=XC[J] + TS[J], w=[f"lruf{j0}"])
                    A(lambda e: e.activation(out=sqb[:, J, 0:N], in_=lruf[:, J, 0:N], func=AF.Square), r=[f"lruf{j0}"], w=[f"sqb{j0}"])

                def p5():
                    ps, psn = next_pb14()
                    for j in range(4):
                        P(lambda e: e.matmul(ps[:, 0:N], lhsT=onesb, rhs=sqb[:, j, 0:N], start=(j == 0), stop=(j == 3)), r=["cstb", "sqb0", "sqb2"], w=[psn])
                    A(lambda e: e.activation(out=slbc[:, 0:N], in_=ps[:, 0:N], func=AF.Ln, bias=EPS, scale=1.0 / 512), r=[psn], w=["slbc"])
                    A(lambda e: e.activation(out=slbc[:, 0:N], in_=slbc[:, 0:N], func=AF.Exp, scale=-0.5), r=["slbc"], w=["slbc"])
                    V(lambda e: e.tensor_tensor(out=lruT[:, :, 0:N], in0=lruf[:, :, 0:N], in1=slbc[:, 0:N].unsqueeze(1).to_broadcast([128, 4, N]), op=ALU.mult),
                      r=["lruf0", "lruf2", "slbc"], w=["lruT"])


                HALVES = ((0, 2), (2, 2))
                if not main:
                    p0()
                    for piece in (p1, p2, p3, p4):
                        for j0_, nj_ in HALVES:
                            piece(j0_, nj_)
                    return

                def tileA(tl):
                    ti = t0 + tl - NT_PRE
                    kcur = ti + 1
                    pt_, ptn = pT[0], "pT0"
                    for kvh in range(2):
                        for kt in range(2):
                            ksl = kcur - 1 + kt
                            which = (2 if ti == 0 else 1) if kt == 0 else 0
                            sc_cnt[0] += 1
                            si = 3 + sc_cnt[0] % 2
                            ps, psn = pb[si], f"pb{si}"
                            P(lambda e: e.matmul(ps[:, 0:512].rearrange("p (g q) -> p g q", g=4), lhsT=kT[kvh * 64:(kvh + 1) * 64, ksl, :],
                                                 rhs=qT[kvh * 64:(kvh + 1) * 64, :, tl * 128:(tl + 1) * 128], start=True, stop=True),
                              r=[f"kT{ksl}", "qT"], w=[psn])
                            eb_, ebn = ebuf[sc_cnt[0] % 2], f"ebuf{sc_cnt[0] % 2}"
                            A(lambda e: e.activation(out=eb_[:], in_=ps[:, 0:512], func=AF.Exp, scale=0.125), r=[psn], w=[ebn])
                            V(lambda e: e.tensor_tensor(out=pt_[:, kt, kvh, :, :], in0=eb_[:].rearrange("p (g q) -> p g q", g=4),
                                                        in1=msk[:, which, kvh * 4:(kvh + 1) * 4, :], op=ALU.mult), r=[ebn, "msk"], w=[f"{ptn}_{kt}{kvh}"])
                    for kvh in range(2):
                        ps, psn = pb[5 + kvh], f"pb{5 + kvh}"
                        for g in range(4):
                            for kt in range(2):
                                ksl = kcur - 1 + kt
                                P(lambda e: e.matmul(ps[:, g * 65:(g + 1) * 65], lhsT=pt_[:, kt, kvh, g, :], rhs=vbuf[:, ksl, kvh, :],
                                                     start=(kt == 0), stop=(kt == 1)), r=[f"{ptn}_{kt}{kvh}", f"v{ksl}", "vbuf"], w=[psn])
                        psv = ps[:, 0:260].rearrange("p (g d) -> p g d", g=4)
                        V(lambda e: e.tensor_tensor(out=zz[:, kvh * 4:(kvh + 1) * 4].unsqueeze(2), in0=psv[:, :, 64:65],
                                                    in1=esink[:, kvh * 4:(kvh + 1) * 4].unsqueeze(2), op=ALU.add), r=[psn, "esink"], w=[f"zz{kvh}"])
                        V(lambda e: e.reciprocal(out=rz[:, kvh * 4:(kvh + 1) * 4], in_=zz[:, kvh * 4:(kvh + 1) * 4]), r=[f"zz{kvh}"], w=[f"rz{kvh}"])
                        V(lambda e: e.tensor_tensor(out=attnf[:, kvh * 256:(kvh + 1) * 256].rearrange("p (g d) -> p g d", g=4), in0=psv[:, :, 0:64],
                                                    in1=rz[:, kvh * 4:(kvh + 1) * 4].unsqueeze(2).to_broadcast([128, 4, 64]), op=ALU.mult),
                          r=[psn, f"rz{kvh}"], w=[f"attnf{kvh}"])
                    A(lambda e: e.activation(out=junk[:], in_=attnf[:], func=AF.Square, accum_out=sml[:, 0:1]), r=["attnf0", "attnf1"], w=["junk", "sml0"])
                    A(lambda e: e.activation(out=sml[:, 1:2], in_=sml[:, 0:1], func=AF.Ln, bias=EPS, scale=1.0 / 512), r=["sml0"], w=["sml1"])
                    A(lambda e: e.activation(out=sml[:, 2:3], in_=sml[:, 1:2], func=AF.Exp, scale=-0.5), r=["sml1"], w=["sml2"])
                    V(lambda e: e.tensor_scalar(out=attnb[:], in0=attnf[:], scalar1=sml[:, 2:3], scalar2=None, op0=ALU.mult),
                      r=["attnf0", "attnf1", "sml2"], w=["attnb"])
                    pbt = pb[0][:].bitcast(BF16)
                    for j in range(4):
                        P(lambda e: e.transpose(out=pbt[:, j * 128:(j + 1) * 128], in_=attnb[:, j * 128:(j + 1) * 128], identity=identb),
                          r=["attnb", "cstb"], w=["pb0"])
                    A(lambda e: e.copy(out=attnT[tl][:].rearrange("p j t -> p (j t)"), in_=pbt[:, 0:512]), r=["pb0"], w=[f"attnT{tl}"])

                def tileB(tl):
                    ti = t0 + tl - NT_PRE
                    zb = zbs[ti % 2]
                    h1f = h1fs[ti % 2]
                    H1, H1B = f"h1f{ti % 2}", f"h1fb{ti % 2}"
                    Z0, Z1 = f"zb{ti % 2}_0", f"zb{ti % 2}_1"
                    xr_, xrn = xres[ti % 2], f"xres{ti % 2}"
                    at_, atn = attnT[tl], f"attnT{tl}"
                    for hv in range(2):
                        ps, psn = (pb[7], "pb7") if (2 * ti + hv) % 2 == 0 else (pb[1], "pb1")
                        for j in range(4):
                            P(lambda e: e.matmul(ps[:, 0:512], lhsT=at_[:, j, :], rhs=wo[:, j, hv * 512:(hv + 1) * 512], start=(j == 0), stop=False),
                              r=[atn, "wo"], w=[psn])
                        for j in range(4):
                            P(lambda e: e.matmul(ps[:, 0:512], lhsT=lruT[:, j, tl * 128:(tl + 1) * 128], rhs=wo[:, 4 + j, hv * 512:(hv + 1) * 512],
                                                 start=False, stop=(j == 3)), r=["lruT", "wo"], w=[psn])
                        V(lambda e: e.scalar_tensor_tensor(out=zb[:, hv * 512:(hv + 1) * 512], in0=xr_[:, hv * 512:(hv + 1) * 512], scalar=ALPHA,
                                                           in1=ps[:, 0:512], op0=ALU.mult, op1=ALU.add), r=[xrn, psn], w=[(Z0, Z1)[hv]])
                        V(lambda e: e.bn_stats(out=stats[:, hv, :], in_=zb[:, hv * 512:(hv + 1) * 512]), r=[(Z0, Z1)[hv]], w=[f"stats{hv}"])
                    V(lambda e: e.bn_aggr(out=mv[:], in_=stats[:].rearrange("p a s -> p (a s)")), r=["stats0", "stats1"], w=["mv"])
                    A(lambda e: e.activation(out=sml[:, 3:4], in_=mv[:, 1:2], func=AF.Ln, bias=EPS, scale=1.0), r=["mv"], w=["sml3"])
                    A(lambda e: e.activation(out=sml[:, 4:5], in_=sml[:, 3:4], func=AF.Exp, scale=-0.5), r=["sml3"], w=["sml4"])
                    V(lambda e: e.scalar_tensor_tensor(out=sml[:, 5:6], in0=mv[:, 0:1], scalar=-1.0, in1=sml[:, 4:5], op0=ALU.mult, op1=ALU.mult),
                      r=["mv", "sml4"], w=["sml5"])
                    A(lambda e: e.activation(out=zb[:], in_=zb[:], func=AF.Identity, bias=sml[:, 5:6], scale=sml[:, 4:5]),
                      r=[Z0, Z1, "sml4", "sml5"], w=[Z0, Z1])
                    V(lambda e: e.tensor_tensor(out=zb[:], in0=zb[:], in1=lnp[:, 0, :], op=ALU.mult), r=[Z0, Z1, "lnp"], w=[Z0, Z1])
                    GP(lambda e: e.tensor_tensor(out=h1f[:, 512:1024], in0=zb[:, 512:1024], in1=lnp[:, 1, 512:1024], op=ALU.add), r=[Z0, Z1, "lnp"], w=[H1B])
                    V(lambda e: e.tensor_tensor(out=h1f[:, 0:512], in0=zb[:, 0:512], in1=lnp[:, 1, 0:512], op=ALU.add), r=[Z0, Z1, "lnp"], w=[H1])
                    A(lambda e: e.copy(out=h1bt[ti % 2][:], in_=h1f[:]), r=[H1, H1B], w=[f"h1bt{ti % 2}"])
                    k.dma("act", f"h1bst{ti % 2}", lambda e: e.dma_start(out=h1bs[ti * 128:(ti + 1) * 128, :], in_=h1bt[ti % 2][:]),
                          reads=[f"h1bt{ti % 2}"], writes=["h1bs"])
                    k.dma("pool", "h1st", lambda e: e.dma_start(out=h1s[ti * 128:(ti + 1) * 128, :], in_=h1f[:]), reads=[H1, H1B], writes=["h1s"])
                    if stage == 1:
                        k.dma("sp", "outst", lambda e: e.dma_start(out=out_d[ti * 128:(ti + 1) * 128, :], in_=h1f[:]), reads=[H1, H1B], writes=["out"])
                    for half in range(2):
                        for c in range(4):
                            cc = half * 4 + c
                            P(lambda e: e.transpose(out=pb[0][:, c * 128:(c + 1) * 128], in_=h1f[:, cc * 128:(cc + 1) * 128], identity=identf[:]),
                              r=[H1, H1B, "identf"], w=["pb0"])
                        A(lambda e: e.copy(out=h1T[:].rearrange("p c t -> p (c t)"), in_=pb[0][:, 0:512]), r=["pb0"], w=["h1T"])
                        for c in range(4):
                            cc = half * 4 + c
                            P(lambda e: e.matmul(pb[6][:, 260:296], lhsT=h1T[:, c, :], rhs=wr[:, cc, :], start=(cc == 0), stop=(cc == 7)),
                              r=["h1T", "wr"], w=["pb6"])
                    V(lambda e: e.tensor_tensor(out=lg_all[:, ti, :], in0=pb[6][:, 260:296], in1=rep[:, 8:44], op=ALU.add), r=["pb6", "rep"], w=["lg_all"])

                def ld_xres(tl):
                    ti = t0 + tl - NT_PRE
                    k.dma("sp", f"ldr{ti % 2}", lambda e: e.dma_start(out=xres[ti % 2][:], in_=xm[ti * 128:(ti + 1) * 128, :]), writes=[f"xres{ti % 2}"])

                p0()
                for j0_, nj_ in HALVES:
                    p1(j0_, nj_)
                tileA(0)
                for j0_, nj_ in HALVES:
                    p2(j0_, nj_)
                tileA(1)
                for j0_, nj_ in HALVES:
                    p3(j0_, nj_)
                tileA(2)
                for j0_, nj_ in HALVES:
                    p4(j0_, nj_)
                tileA(3)
                p5()
                ld_xres(0)
                ld_xres(1)
                for tl in range(G):
                    tileB(tl)
                    if tl + 2 < G:
                        ld_xres(tl + 2)

            for grp in GROUPS:
                do_group(*grp)
            k.barrier()

        if stage == 1:
            k.barrier()
            return nc

        T = NT_MAIN
        gates = sbt(st0, "gates", [128, 2, T], F32)
        dest_i = sbt(st0, "dest_i", [128, 2, T], I32)
        idxw = sbt(st0, "idxw", [128, 2, NB], I32)
        pcol = sbt(st0, "pcol", [128, 2], F32)
        k.dma("sp", "c0", lambda e: e.dma_start(out=pcol[:], in_=pcol_d), writes=["pcol"])
        NWB = 3
        wg = [sbt(st0, f"wg{i}", [128, 8, 512], BF16) for i in range(NWB)]
        wu = [sbt(st0, f"wu{i}", [128, 8, 512], BF16) for i in range(NWB)]
        wd = [sbt(st0, f"wd{i}", [128, 4, D], BF16) for i in range(NWB)]
        xb = [sbt(st0, f"xb{i}", [128, D], BF16) for i in range(3)]
        xbT = [sbt(st0, f"xbT{i}", [128, 8, 128], BF16) for i in range(3)]
        sgm = sbt(st0, "sgm", [128, 512], F32)
        gsm = sbt(st0, "gsm", [128, 512], F32)
        ab = [sbt(st0, f"ab{i}", [128, 512], BF16) for i in range(2)]
        aT = [sbt(st0, f"aT{i}", [128, 4, 128], BF16) for i in range(2)]
        yb = [sbt(st0, f"yb{i}", [128, D], F32) for i in range(2)]
        y0 = [sbt(st0, f"y0_{i}", [128, D], F32) for i in range(2)]
        y1 = [sbt(st0, f"y1_{i}", [128, D], F32) for i in range(2)]
        hr = [sbt(st0, f"hr{i}", [128, D], F32) for i in range(2)]
        zcb = [sbt(st0, f"zc{i}", [128, D], F32) for i in range(3)]
        zn = [sbt(st0, f"zn{i}", [128, D], F32) for i in range(2)]
        oc = [sbt(st0, f"oc{i}", [128, D], F32) for i in range(2)]
        stats2 = [sbt(st0, f"stats2_{i}", [128, 2, 6], F32) for i in range(2)]
        mv2 = [sbt(st0, f"mv2_{i}", [128, 2], F32) for i in range(2)]
        sm2 = [sbt(st0, f"sm2_{i}", [128, 4], F32) for i in range(2)]
        lnp2 = sbt(st0, "lnp2", [128, 2, D], F32)
        k.dma("sp", "c2", lambda e: e.dma_start(out=lnp2[:].rearrange("p a d -> p (a d)"), in_=ln_d[:, 2 * D:4 * D]), writes=["lnp2"])

        bnd_reg = nc.gpsimd.alloc_register("wbound")
        nc.gpsimd.reg_mov(bnd_reg, 32 * 256 - 1)

        def issue_weights(pos):
            sbk, ws = sb_order[pos], pos % NWB
            for wt, wsrc, wn in ((wg, w_gate_d, "wg"), (wu, w_up_d, "wu"), (wd, w_down_d, "wd")):
                dst = wt[ws][:].rearrange("p c f -> p (c f)")
                for a in range(2):
                    k.dma("pool", f"wq{wn}{ws}_{a}", lambda e: e.indirect_dma_start(out=dst[:, a * 2048:(a + 1) * 2048], out_offset=None, in_=wsrc,
                                                                                 in_offset=bass.IndirectOffsetOnAxis(ap=idxw[:, a, sbk:sbk + 1], axis=0),
                                                                                 bounds_check=bnd_reg, oob_is_err=False),
                          reads=["idxw"], writes=[f"{wn}{ws}_{a}"])

        sb_order = []
        for i_ in range(NB // 3):
            sb_order += [i_, NB // 3 + i_, NB - 1 - i_]

        with contextlib.ExitStack() as sb_:
            def t512(name, dt=F32):
                return sbt(sb_, name, [128, 512], dt)

            gmax = sbt(sb_, "gmax", [128, T], F32)
            goh = sbt(sb_, "goh", [128, T, 4], F32)
            gd = sbt(sb_, "gd", [128, T, 4], F32)
            gs = sbt(sb_, "gs", [128, T], F32)
            gw = sbt(sb_, "gw", [128, T], F32)
            tmp = t512("tmp")
            sel = sbt(sb_, "sel", [128, T, 8], F32)
            sel2 = sbt(sb_, "sel2", [128, T, 8], F32)
            m1 = sbt(sb_, "m1", [128, T], F32)
            m2 = sbt(sb_, "m2", [128, T], F32)
            oh1 = sbt(sb_, "oh1", [128, T, 8], F32)
            oh2 = sbt(sb_, "oh2", [128, T, 8], F32)
            dm = sbt(sb_, "dm", [128, T], F32)
            OH1 = t512("OH1")
            OH2 = t512("OH2")
            OHb = t512("OHb", BF16)
            tot_et = t512("tot_et")
            flags = t512("flags")
            cum_et = t512("cum_et")
            excl_et = t512("excl_et")
            base = t512("base")
            cmp16 = t512("cmp16")
            nblk = sbt(sb_, "nblk", [128, 32], F32)
            pendb = sbt(sb_, "pendb", [128, 32], F32)
            pstart = sbt(sb_, "pstart", [128, 32], F32)
            ones32 = sbt(sb_, "ones32", [128, 32], F32)
            dd = sbt(sb_, "dd", [128, 2, T], F32)
            cmpb = sbt(sb_, "cmpb", [128, NB, 32], F32)
            ebf = sbt(sb_, "ebf", [128, NB], F32)

            gl = lg_all[:, :, 0:4]
            el4 = lg_all[:, :, 4:36].rearrange("p t (g e) -> p t g e", g=4)

            def bc(ap, axis, shape):
                return ap.unsqueeze(axis).to_broadcast(shape)

            V(lambda e: e.tensor_reduce(out=gmax[:], in_=gl, axis=AX.X, op=ALU.max), r=["lg_all"], w=["gmax"])
            V(lambda e: e.tensor_tensor(out=goh[:], in0=gl, in1=bc(gmax[:], 2, [128, T, 4]), op=ALU.is_equal), r=["lg_all", "gmax"], w=["goh"])
            V(lambda e: e.tensor_tensor(out=gd[:], in0=gl, in1=bc(gmax[:], 2, [128, T, 4]), op=ALU.subtract), r=["lg_all", "gmax"], w=["gd"])
            A(lambda e: e.activation(out=gd[:], in_=gd[:], func=AF.Exp), r=["gd"], w=["gd"])
            V(lambda e: e.tensor_reduce(out=gs[:], in_=gd[:], axis=AX.X, op=ALU.add), r=["gd"], w=["gs"])
            V(lambda e: e.reciprocal(out=gw[:], in_=gs[:]), r=["gs"], w=["gw"])
            tmp4 = tmp[:].rearrange("p (t g e) -> p t g e", t=T, g=4)
            V(lambda e: e.tensor_tensor(out=tmp4, in0=el4, in1=bc(goh[:], 3, [128, T, 4, 8]), op=ALU.mult), r=["lg_all", "goh"], w=["tmp"])
            V(lambda e: e.tensor_reduce(out=sel[:], in_=tmp[:].rearrange("p (t g e) -> p t e g", t=T, g=4), axis=AX.X, op=ALU.add), r=["tmp"], w=["sel"])
            V(lambda e: e.tensor_reduce(out=m1[:], in_=sel[:], axis=AX.X, op=ALU.max), r=["sel"], w=["m1"])
            V(lambda e: e.tensor_tensor(out=oh1[:], in0=sel[:], in1=bc(m1[:], 2, [128, T, 8]), op=ALU.is_equal), r=["sel", "m1"], w=["oh1"])
            V(lambda e: e.scalar_tensor_tensor(out=sel2[:], in0=oh1[:], scalar=-1e30, in1=sel[:], op0=ALU.mult, op1=ALU.add), r=["oh1", "sel"], w=["sel2"])
            V(lambda e: e.tensor_reduce(out=m2[:], in_=sel2[:], axis=AX.X, op=ALU.max), r=["sel2"], w=["m2"])
            V(lambda e: e.tensor_tensor(out=oh2[:], in0=sel2[:], in1=bc(m2[:], 2, [128, T, 8]), op=ALU.is_equal), r=["sel2", "m2"], w=["oh2"])
            V(lambda e: e.tensor_tensor(out=dm[:], in0=m2[:], in1=m1[:], op=ALU.subtract), r=["m1", "m2"], w=["dm"])
            A(lambda e: e.activation(out=dm[:], in_=dm[:], func=AF.Exp), r=["dm"], w=["dm"])
            V(lambda e: e.tensor_scalar(out=dm[:], in0=dm[:], scalar1=1.0, scalar2=None, op0=ALU.add), r=["dm"], w=["dm"])
            V(lambda e: e.reciprocal(out=dm[:], in_=dm[:]), r=["dm"], w=["dm"])
            V(lambda e: e.tensor_tensor(out=gates[:, 0, :], in0=gw[:], in1=dm[:], op=ALU.mult), r=["gw", "dm"], w=["gates0"])
            V(lambda e: e.tensor_tensor(out=gates[:, 1, :], in0=gw[:], in1=gates[:, 0, :], op=ALU.subtract), r=["gw", "gates0"], w=["gates1"])
            OH1v = OH1[:].rearrange("p (t g e) -> p t g e", t=T, g=4)
            OH2v = OH2[:].rearrange("p (t g e) -> p t g e", t=T, g=4)
            V(lambda e: e.tensor_tensor(out=OH1v, in0=bc(goh[:], 3, [128, T, 4, 8]), in1=bc(oh1[:], 2, [128, T, 4, 8]), op=ALU.mult), r=["goh", "oh1"], w=["OH1"])
            V(lambda e: e.tensor_tensor(out=OH2v, in0=bc(goh[:], 3, [128, T, 4, 8]), in1=bc(oh2[:], 2, [128, T, 4, 8]), op=ALU.mult), r=["goh", "oh2"], w=["OH2"])
            V(lambda e: e.tensor_tensor(out=OHb[:], in0=OH1[:], in1=OH2[:], op=ALU.add), r=["OH1", "OH2"], w=["OHb"])
            P(lambda e: e.matmul(pb[1][:, 0:512], lhsT=lstrict, rhs=OHb[:], start=True, stop=True), r=["cstb", "OHb"], w=["pb1"])
            P(lambda e: e.matmul(pb[2][:, 0:512], lhsT=onesb, rhs=OHb[:], start=True, stop=True), r=["cstb", "OHb"], w=["pb2"])
            tot3 = tot_et[:].rearrange("p (e t) -> p e t", e=32)
            V(lambda e: e.tensor_copy(out=tot3, in_=pb[2][:, 0:512].rearrange("p (t e) -> p e t", e=32)), r=["pb2"], w=["tot_et"])
            V(lambda e: e.memset(flags[:], 1.0), w=["flags"])
            V(lambda e: e.memset(flags[:].rearrange("p (e t) -> p e t", e=32)[:, :, 0:1], 0.0), r=["flags"], w=["flags"])
            V(lambda e: e.tensor_tensor_scan(out=cum_et[:], data0=flags[:], data1=tot_et[:], initial=0.0, op0=ALU.mult, op1=ALU.add),
              r=["flags", "tot_et"], w=["cum_et"])
            V(lambda e: e.tensor_tensor(out=excl_et[:], in0=cum_et[:], in1=tot_et[:], op=ALU.subtract), r=["cum_et", "tot_et"], w=["excl_et"])
            cnt_e = cum_et[:].rearrange("p (e t) -> p e t", e=32)[:, :, T - 1:T]
            c16 = cmp16[:].rearrange("p (e j) -> p e j", e=32)
            V(lambda e: e.tensor_tensor(out=c16, in0=cnt_e.to_broadcast([128, 32, 16]), in1=bc(rep[:, 44:60], 1, [128, 32, 16]), op=ALU.is_gt),
              r=["cum_et", "rep"], w=["cmp16"])
            V(lambda e: e.tensor_reduce(out=nblk[:], in_=c16, axis=AX.X, op=ALU.add), r=["cmp16"], w=["nblk"])
            V(lambda e: e.memset(ones32[:], 1.0), w=["ones32"])
            V(lambda e: e.tensor_tensor_scan(out=pendb[:], data0=ones32[:], data1=nblk[:], initial=0.0, op0=ALU.mult, op1=ALU.add),
              r=["ones32", "nblk"], w=["pendb"])
            V(lambda e: e.tensor_tensor(out=pstart[:], in0=pendb[:], in1=nblk[:], op=ALU.subtract), r=["pendb", "nblk"], w=["pstart"])
            V(lambda e: e.tensor_scalar(out=pstart[:], in0=pstart[:], scalar1=float(SBK), scalar2=None, op0=ALU.mult), r=["pstart"], w=["pstart"])
            V(lambda e: e.tensor_scalar(out=pendb[:], in0=pendb[:], scalar1=float(SBK), scalar2=None, op0=ALU.mult), r=["pendb"], w=["pendb"])
            base3 = base[:].rearrange("p (t e) -> p t e", e=32)
            V(lambda e: e.tensor_tensor(out=base3, in0=pb[1][:, 0:512].rearrange("p (t e) -> p t e", e=32),
                                        in1=excl_et[:].rearrange("p (e t) -> p t e", e=32), op=ALU.add), r=["pb1", "excl_et"], w=["base"])
            V(lambda e: e.tensor_tensor(out=base3, in0=base3, in1=bc(pstart[:], 1, [128, T, 32]), op=ALU.add), r=["base", "pstart"], w=["base"])
            for kk, OHk in enumerate((OH1, OH2)):
                V(lambda e: e.tensor_tensor(out=tmp[:], in0=OHk[:], in1=base[:], op=ALU.mult), r=[f"OH{kk + 1}", "base"], w=["tmp"])
                V(lambda e: e.tensor_reduce(out=dd[:, kk, :], in_=tmp[:].rearrange("p (t e) -> p t e", e=32), axis=AX.X, op=ALU.add), r=["tmp"], w=[f"dd{kk}"])
            V(lambda e: e.tensor_copy(out=dest_i[:], in_=dd[:]), r=["dd0", "dd1"], w=["dest_i"])
            V(lambda e: e.tensor_tensor(out=cmpb[:], in0=bc(pendb[:], 1, [128, NB, 32]), in1=bc(rep[:, 60:60 + NB], 2, [128, NB, 32]), op=ALU.is_le),
              r=["pendb", "rep"], w=["cmpb"])
            V(lambda e: e.tensor_reduce(out=ebf[:], in_=cmpb[:], axis=AX.X, op=ALU.add), r=["cmpb"], w=["ebf"])
            V(lambda e: e.tensor_scalar(out=ebf[:], in0=ebf[:], scalar1=31.0, scalar2=0.0, op0=ALU.min, op1=ALU.max), r=["ebf"], w=["ebf"])
            idxwf = sbt(sb_, "idxwf", [128, 2, NB], F32)
            validb = sbt(sb_, "validb", [128, NB], F32)
            V(lambda e: e.tensor_scalar(out=validb[:], in0=rep[:, 60:60 + NB], scalar1=pendb[:, 31:32], scalar2=None, op0=ALU.is_lt), r=["rep", "pendb"], w=["validb"])
            V(lambda e: e.tensor_scalar(out=ebf[:], in0=ebf[:], scalar1=-4000.0, scalar2=None, op0=ALU.add), r=["ebf"], w=["ebf"])
            V(lambda e: e.tensor_tensor(out=ebf[:], in0=ebf[:], in1=validb[:], op=ALU.mult), r=["ebf", "validb"], w=["ebf"])
            V(lambda e: e.tensor_scalar(out=ebf[:], in0=ebf[:], scalar1=4000.0, scalar2=None, op0=ALU.add), r=["ebf"], w=["ebf"])
            for a in range(2):
                V(lambda e: e.tensor_scalar(out=idxwf[:, a, :], in0=ebf[:], scalar1=256.0, scalar2=pcol[:, a:a + 1], op0=ALU.mult, op1=ALU.add),
                  r=["ebf", "pcol"], w=[f"idxwf{a}"])
            V(lambda e: e.tensor_copy(out=idxw[:], in_=idxwf[:]), r=["idxwf0", "idxwf1"], w=["idxw"])
            for pos in range(NWB):
                issue_weights(pos)
            hbt = [sbt(sb_, f"hbt{i}", [128, D], BF16) for i in range(4)]
            for t in range(T):
                k.dma("sp", f"ldhb{t % 4}", lambda e: e.dma_start(out=hbt[t % 4][:], in_=h1bs[t * 128:(t + 1) * 128, :]), reads=["h1bs"], writes=[f"hbt{t % 4}"])
                for kk in range(2):
                    k.dma("pool", f"xsc{t % 4}_{kk}", lambda e: e.indirect_dma_start(out=Xs, out_offset=bass.IndirectOffsetOnAxis(ap=dest_i[:, kk, t:t + 1], axis=0),
                                                                                     in_=hbt[t % 4][:], in_offset=None),
                          reads=[f"hbt{t % 4}", "dest_i"], writes=[f"Xs_sc{t % 4}_{kk}"])
            XS_ALL = [f"Xs_sc{i}_{kk}" for i in range(4) for kk in range(2)]

        with contextlib.ExitStack() as sd:
            NQ = NB * 2
            def rows_of(q):
                return (sb_order[q // 2] * 2 + q % 2) * 128

            def moe_s1(q):
                sbk, s3 = sb_order[q // 2], q % 3
                ws = (q // 2) % NWB
                if q % 2 == 0 and q // 2 >= NWB:
                    issue_weights(q // 2)
                for qq in ((0, 1, 2) if q == 0 else (q + 2,)):
                    if qq < NQ:
                        k.dma("sp", f"ldxb{qq % 3}", lambda e: e.dma_start(out=xb[qq % 3][:], in_=Xs[rows_of(qq):rows_of(qq) + 128, :]),
                              reads=XS_ALL, writes=[f"xb{qq % 3}"])
                pbt = pb[0][:].bitcast(BF16)
                for c in range(8):
                    P(lambda e: e.transpose(out=pbt[:, c * 128:(c + 1) * 128], in_=xb[s3][:, c * 128:(c + 1) * 128], identity=identb),
                      r=[f"xb{s3}", "cstb"], w=["pb0"])
                A(lambda e: e.copy(out=xbT[s3][:].rearrange("p c t -> p (c t)"), in_=pbt[:, 0:1024]), r=["pb0"], w=[f"xbT{s3}"])

            def moe_s2(q):
                ws, s3, s2 = (q // 2) % NWB, q % 3, q % 2
                pg, pgn = pb[1 + 2 * s2], f"pb{1 + 2 * s2}"
                pu, pun = pb[2 + 2 * s2], f"pb{2 + 2 * s2}"
                for c in range(8):
                    P(lambda e: e.matmul(pg[:, 0:512], lhsT=xbT[s3][:, c, :], rhs=wg[ws][:, c, :], start=(c == 0), stop=(c == 7)),
                      r=[f"xbT{s3}", f"wg{ws}_0", f"wg{ws}_1"], w=[pgn])
                for c in range(8):
                    P(lambda e: e.matmul(pu[:, 0:512], lhsT=xbT[s3][:, c, :], rhs=wu[ws][:, c, :], start=(c == 0), stop=(c == 7)),
                      r=[f"xbT{s3}", f"wu{ws}_0", f"wu{ws}_1"], w=[pun])
                A(lambda e: e.activation(out=sgm[:], in_=pg[:, 0:512], func=AF.Sigmoid), r=[pgn], w=["sgm"])
                V(lambda e: e.tensor_tensor(out=gsm[:], in0=sgm[:], in1=pg[:, 0:512], op=ALU.mult), r=["sgm", pgn], w=["gsm"])
                V(lambda e: e.tensor_tensor(out=ab[s2][:], in0=gsm[:], in1=pu[:, 0:512], op=ALU.mult), r=["gsm", pun], w=[f"ab{s2}"])

            def moe_s3(q):
                s2 = q % 2
                pbt5 = pb[5][:].bitcast(BF16)
                for c in range(4):
                    P(lambda e: e.transpose(out=pbt5[:, c * 128:(c + 1) * 128], in_=ab[s2][:, c * 128:(c + 1) * 128], identity=identb),
                      r=[f"ab{s2}", "cstb"], w=["pb5"])
                A(lambda e: e.copy(out=aT[s2][:].rearrange("p c t -> p (c t)"), in_=pbt5[:, 0:512]), r=["pb5"], w=[f"aT{s2}"])

            def moe_s4(q):
                ws, s2 = (q // 2) % NWB, q % 2
                for hv in range(2):
                    py, pyn = pb[6 + hv], f"pb{6 + hv}"
                    for c in range(4):
                        P(lambda e: e.matmul(py[:, 0:512], lhsT=aT[s2][:, c, :], rhs=wd[ws][:, c, hv * 512:(hv + 1) * 512], start=(c == 0), stop=(c == 3)),
                          r=[f"aT{s2}", f"wd{ws}_0", f"wd{ws}_1"], w=[pyn])
                    A(lambda e: e.copy(out=yb[s2][:, hv * 512:(hv + 1) * 512], in_=py[:, 0:512]), r=[pyn], w=[f"yb{s2}{'ab'[hv]}"])
                k.dma("act", f"yst{s2}", lambda e: e.dma_start(out=Ys[rows_of(q):rows_of(q) + 128, :], in_=yb[s2][:]), reads=[f"yb{s2}a", f"yb{s2}b"], writes=[f"Ys{s2}"])

            for i in range(NQ + 3):
                if i < NQ:
                    moe_s1(i)
                if 0 <= i - 1 < NQ:
                    moe_s2(i - 1)
                if 0 <= i - 2 < NQ:
                    moe_s3(i - 2)
                if 0 <= i - 3 < NQ:
                    moe_s4(i - 3)

            def comb_loads(t):
                s = t % 2
                k.dma("pool", f"g0_{s}", lambda e: e.indirect_dma_start(out=y0[s][:], out_offset=None, in_=Ys,
                                                                        in_offset=bass.IndirectOffsetOnAxis(ap=dest_i[:, 0, t:t + 1], axis=0)),
                      reads=["Ys0", "Ys1", "dest_i"], writes=[f"y0_{s}"])
                k.dma("pool", f"g1_{s}", lambda e: e.indirect_dma_start(out=y1[s][:], out_offset=None, in_=Ys,
                                                                        in_offset=bass.IndirectOffsetOnAxis(ap=dest_i[:, 1, t:t + 1], axis=0)),
                      reads=["Ys0", "Ys1", "dest_i"], writes=[f"y1_{s}"])
                k.dma("sp", f"ldh{s}", lambda e: e.dma_start(out=hr[s][:], in_=h1s[t * 128:(t + 1) * 128, :]), reads=["h1s"], writes=[f"hr{s}"])

            def comb_s1(t):
                s = t % 2
                A(lambda e: e.activation(out=zcb[t % 3][:], in_=y0[s][:], func=AF.Copy, scale=gates[:, 0, t:t + 1]), r=[f"y0_{s}", "gates0"], w=[f"zc{t % 3}"])
                V(lambda e: e.scalar_tensor_tensor(out=zcb[t % 3][:], in0=y1[s][:], scalar=gates[:, 1, t:t + 1], in1=zcb[t % 3][:], op0=ALU.mult, op1=ALU.add),
                  r=[f"y1_{s}", "gates1", f"zc{t % 3}"], w=[f"zc{t % 3}"])
                V(lambda e: e.scalar_tensor_tensor(out=zcb[t % 3][:], in0=hr[s][:], scalar=ALPHA, in1=zcb[t % 3][:], op0=ALU.mult, op1=ALU.add), r=[f"hr{s}", f"zc{t % 3}"], w=[f"zc{t % 3}"])
                for hv in range(2):
                    V(lambda e: e.bn_stats(out=stats2[s][:, hv, :], in_=zcb[t % 3][:, hv * 512:(hv + 1) * 512]), r=[f"zc{t % 3}"], w=[f"st2{hv}_{s}"])
                V(lambda e: e.bn_aggr(out=mv2[s][:], in_=stats2[s][:].rearrange("p a s -> p (a s)")), r=[f"st20_{s}", f"st21_{s}"], w=[f"mv2_{s}"])

            def comb_s2(t):
                s = t % 2
                A(lambda e: e.activation(out=sm2[s][:, 0:1], in_=mv2[s][:, 1:2], func=AF.Ln, bias=EPS, scale=1.0), r=[f"mv2_{s}"], w=[f"sm2a{s}"])
                A(lambda e: e.activation(out=sm2[s][:, 1:2], in_=sm2[s][:, 0:1], func=AF.Exp, scale=-0.5), r=[f"sm2a{s}"], w=[f"sm2b{s}"])
                V(lambda e: e.scalar_tensor_tensor(out=sm2[s][:, 2:3], in0=mv2[s][:, 0:1], scalar=-1.0, in1=sm2[s][:, 1:2], op0=ALU.mult, op1=ALU.mult),
                  r=[f"mv2_{s}", f"sm2b{s}"], w=[f"sm2c{s}"])
                A(lambda e: e.activation(out=zn[s][:], in_=zcb[t % 3][:], func=AF.Identity, bias=sm2[s][:, 2:3], scale=sm2[s][:, 1:2]),
                  r=[f"zc{t % 3}", f"sm2b{s}", f"sm2c{s}"], w=[f"zn{s}"])

            def comb_s3(t):
                s = t % 2
                V(lambda e: e.tensor_tensor(out=zn[s][:], in0=zn[s][:], in1=lnp2[:, 0, :], op=ALU.mult), r=[f"zn{s}", "lnp2"], w=[f"zn{s}"])
                V(lambda e: e.tensor_tensor(out=oc[s][:], in0=zn[s][:], in1=lnp2[:, 1, :], op=ALU.add), r=[f"zn{s}", "lnp2"], w=[f"oc{s}"])
                k.dma("sp", f"outst{s}", lambda e: e.dma_start(out=out_d[t * 128:(t + 1) * 128, :], in_=oc[s][:]), reads=[f"oc{s}"], writes=["out"])

            comb_loads(0)
            comb_loads(1)
            comb_s1(0)
            for t in range(NT_MAIN):
                if t + 1 < NT_MAIN:
                    comb_s1(t + 1)
                if t + 2 < NT_MAIN:
                    comb_loads(t + 2)
                comb_s2(t)
                comb_s3(t)
            k.barrier()
    return nc


def _host_constants():
    kk = np.arange(128, dtype=np.float32)[:, None]
    qq = np.arange(128, dtype=np.float32)[None, :]
    slopes = np.exp2(-8.0 * np.arange(1, 9, dtype=np.float32) / 8.0).astype(np.float32)
    cur = np.zeros((128, 8, 128), np.float32)
    prev = np.zeros((128, 8, 128), np.float32)
    for h in range(8):
        cur[:, h, :] = (kk <= qq) * np.exp(-slopes[h] * (qq - kk))
        prev[:, h, :] = (kk > qq) * np.exp(-slopes[h] * (qq - kk + 128.0))
    ident = np.eye(128, dtype=np.float32)
    lstrict = (kk < qq).astype(np.float32)
    ones = np.ones((128, 128), np.float32)
    cf = np.concatenate([ident, ident, lstrict, ones], axis=1)
    return cur, prev, cf


def kernel(x, meta_tokens, w_in, conv_w, conv_b, lru_wa, lru_ba, lru_wx, lru_bx, lru_lambda,
           attn_sinks, g_attn, g_lru, w_out, ln1_g, ln1_b, w_group, b_group, w_router,
           b_router, w_gate, w_up, w_down, ln2_g, ln2_b, _stage=3):
    f32 = np.float32
    x = np.asarray(x, f32)
    B = x.shape[0]
    cur, prev, cf = _host_constants()
    w_in0 = np.asarray(w_in, f32)[0]
    qcols = w_in0[:, 0:512].reshape(D, 2, 4, 64).transpose(0, 2, 1, 3).reshape(D, 512)
    w_in_p = np.ascontiguousarray(np.concatenate([qcols, w_in0[:, 512:]], axis=1))
    wbd = np.zeros((128, 2, 4, 128), f32)
    for wi, wsrc in enumerate((np.asarray(lru_wa, f32)[0], np.asarray(lru_wx, f32)[0])):
        for blk in range(8):
            j, half = blk // 2, blk % 2
            wbd[half * 64:(half + 1) * 64, wi, j, half * 64:(half + 1) * 64] = wsrc[blk]
    wbd = wbd.reshape(128, -1)

    def pc(v):
        return np.asarray(v, f32).reshape(-1, 128).T

    cw = np.asarray(conv_w, f32)[0]
    cwp = np.stack([pc(cw[kk]) for kk in range(4)], axis=2).reshape(128, 16)
    gcat = np.concatenate([np.asarray(g_attn, f32)[0], np.asarray(g_lru, f32)[0]])
    pp = np.concatenate([cwp, pc(np.asarray(conv_b, f32)[0]), pc(np.asarray(lru_ba, f32)[0].reshape(-1)),
                         pc(np.asarray(lru_bx, f32)[0].reshape(-1)), pc(np.asarray(lru_lambda, f32)[0]), pc(gcat)], axis=1)
    pp = np.ascontiguousarray(pp, f32)
    biases = np.concatenate([np.asarray(b_group, f32)[0], np.asarray(b_router, f32)[0].reshape(-1)])
    rep_row = np.concatenate([np.asarray(attn_sinks, f32)[0], biases, float(SBK) * np.arange(16, dtype=f32), float(SBK) * np.arange(64, dtype=f32)])
    rep = np.ascontiguousarray(np.broadcast_to(rep_row[None, :], (128, rep_row.size)), f32)
    ln_row = np.concatenate([np.asarray(a, f32)[0] for a in (ln1_g, ln1_b, ln2_g, ln2_b)])
    ln = np.ascontiguousarray(np.broadcast_to(ln_row[None, :], (128, ln_row.size)), f32)
    w_r = np.ascontiguousarray(np.concatenate([np.asarray(w_group, f32)[0],
                                               np.asarray(w_router, f32)[0].transpose(1, 0, 2).reshape(D, 32)], axis=1), f32)
    w_out0 = np.ascontiguousarray(np.asarray(w_out, f32)[0])
    def relayout(w):
        e_, kdim, fdim = w.shape
        r = w.reshape(e_, kdim // 128, 128, fdim).transpose(0, 2, 1, 3)
        return np.ascontiguousarray(r).reshape(e_ * 256, 2048)

    wg0 = relayout(np.asarray(w_gate, f32)[0])
    wu0 = relayout(np.asarray(w_up, f32)[0])
    wd0 = relayout(np.asarray(w_down, f32)[0])
    pcolv = np.ascontiguousarray(np.stack([2.0 * np.arange(128), 2.0 * np.arange(128) + 1.0], axis=1), f32)
    meta = np.asarray(meta_tokens, f32)

    in_maps = []
    for b in range(B):
        seq = np.concatenate([np.zeros((112, D), f32), meta, x[b]], axis=0)
        for hf in range(2):
            if hf == 1:
                xs = seq
                nfake = 112
                xmain = x[b, 2048:4096]
            else:
                xs = np.concatenate([np.zeros((16 * 128, D), f32), seq[0:17 * 128]], axis=0)
                nfake = 16 * 128 + 112
                xmain = x[b, 0:2048]
            tm = np.ones((NT_PRE * 128,), f32)
            tm[:nfake] = 0.0
            prev0 = prev.copy()
            if hf == 0:
                prev0[0:112] = 0.0
            masks = np.stack([cur, prev, prev0], axis=1).reshape(128, -1)
            in_maps.append({
                "xsT": np.ascontiguousarray(xs.T), "xm": np.ascontiguousarray(xmain),
                "tmask": np.ascontiguousarray(np.broadcast_to(tm[None, :], (128, tm.size))),
                "masks": np.ascontiguousarray(masks), "w_in": w_in_p, "w_out": w_out0, "wbd": wbd, "pp": pp, "rep": rep,
                "ln": ln, "w_r": w_r, "cf": cf, "w_gate": wg0, "w_up": wu0, "w_down": wd0, "pcol": pcolv,
            })
    nc = build_program(stage=_stage)
    res = run_bass_kernel_spmd(nc, in_maps, core_ids=list(range(2 * B)))
    out = np.empty((B, 4096, D), f32)
    for b in range(B):
        for hf in range(2):
            out[b, hf * 2048:(hf + 1) * 2048] = res.results[b * 2 + hf]["out"]
    return out
```
